# Optimizing a Trainium2 kernel written in Bass

```python
import jax
import jax.numpy as jnp
from jax import lax
import numpy as np

D_MODEL = 1024
BATCH = 8
SEQ = 2048
DEPTH = 1

HEAD_DIM = 64
A_HEADS = 8
A_KV_HEADS = 2
B_HEADS = 8
B_KV_HEADS = 2
WINDOW = 128
BLOCK = 128
GRID_W = 64
ROPE_THETA = 10000.0
N_EXPERTS = 32
TOP_K = 4
D_FF = 1024
MOE_BLOCK = 128
SWIGLU_LIMIT = 7.0
SWIGLU_ALPHA = 1.702
LN_EPS = 1e-5
RMS_EPS = 1e-6
NEG_INF = -1e30
DN_ALPHA = (2.0 * DEPTH) ** 0.25
DN_BETA = (8.0 * DEPTH) ** -0.25

A_Q = A_HEADS * HEAD_DIM
A_KV = A_KV_HEADS * HEAD_DIM
B_Q = B_HEADS * HEAD_DIM
B_KV = B_KV_HEADS * HEAD_DIM
IN_SPLITS = (A_Q, A_KV, A_KV, B_Q, B_KV, B_KV, D_MODEL, D_MODEL)
IN_WIDTH = sum(IN_SPLITS)
IN_OFFSETS = tuple(int(o) for o in np.cumsum(IN_SPLITS)[:-1])
ALIBI_SLOPES = tuple(2.0 ** (-8.0 * (h + 1) / A_HEADS) for h in range(A_HEADS))

kernel_name = "hybrid_window_grid_attn_moe_deepnorm"


def layer_norm(x, g, b):
    xf = x.astype(jnp.float32)
    mu = jnp.mean(xf, -1, keepdims=True)
    var = jnp.mean(jnp.square(xf - mu), -1, keepdims=True)
    y = (xf - mu) * lax.rsqrt(var + LN_EPS) * g.astype(jnp.float32) + b.astype(jnp.float32)
    return y.astype(x.dtype)


def rms_norm(x, g):
    xf = x.astype(jnp.float32)
    y = xf * lax.rsqrt(jnp.mean(xf * xf, -1, keepdims=True) + RMS_EPS) * g.astype(jnp.float32)
    return y.astype(x.dtype)


def rope_1d(xs, pos):
    dim = xs.shape[-1]
    half = dim // 2
    inv = ROPE_THETA ** (-jnp.arange(half, dtype=jnp.float32) * (2.0 / dim))
    ang = pos.astype(jnp.float32)[:, None] * inv[None, :]
    cos = jnp.cos(ang)[None, :, None, :]
    sin = jnp.sin(ang)[None, :, None, :]
    x1 = xs[..., :half].astype(jnp.float32)
    x2 = xs[..., half:].astype(jnp.float32)
    return jnp.concatenate([x1 * cos - x2 * sin, x2 * cos + x1 * sin], -1)


def axial_rope(x, row, col):
    half = x.shape[-1] // 2
    y = jnp.concatenate([rope_1d(x[..., :half], row), rope_1d(x[..., half:], col)], -1)
    return y.astype(x.dtype)


def window_attention(q, k, v, sink):
    bsz, seq, n_heads, hd = q.shape
    n_kv = k.shape[2]
    grp = n_heads // n_kv
    nb = seq // BLOCK
    qb = q.reshape(bsz, nb, BLOCK, n_kv, grp, hd)

    def band(t):
        tp = jnp.pad(t, ((0, 0), (BLOCK, BLOCK), (0, 0), (0, 0)))
        tp = tp.reshape(bsz, nb + 2, BLOCK, n_kv, hd)
        return jnp.concatenate([tp[:, :-2], tp[:, 1:-1], tp[:, 2:]], axis=2)

    kb = band(k)
    vb = band(v)
    s = jnp.einsum('bnqkgd,bnskd->bnkgqs', qb, kb).astype(jnp.float32) * (hd ** -0.5)
    blk = jnp.arange(nb)[:, None] * BLOCK
    q_pos = blk + jnp.arange(BLOCK)[None, :]
    k_pos = blk - BLOCK + jnp.arange(3 * BLOCK)[None, :]
    dist_i = jnp.abs(q_pos[:, :, None] - k_pos[:, None, :])
    valid = (dist_i <= WINDOW) & (k_pos[:, None, :] >= 0) & (k_pos[:, None, :] < seq)
    dist = dist_i.astype(jnp.float32)
    slopes = jnp.asarray(ALIBI_SLOPES, jnp.float32).reshape(n_kv, grp)
    bias = jnp.where(valid[:, None, None], -slopes[None, :, :, None, None] * dist[:, None, None], NEG_INF)
    s = s + bias[None]
    sk = sink.astype(jnp.float32).reshape(1, 1, n_kv, grp, 1, 1)
    m = jnp.maximum(jnp.max(s, -1, keepdims=True), sk)
    p = jnp.exp(s - m)
    p = p / (jnp.sum(p, -1, keepdims=True) + jnp.exp(sk - m))
    o = jnp.einsum('bnkgqs,bnskd->bnqkgd', p.astype(v.dtype), vb)
    return o.reshape(bsz, seq, n_heads * hd)


def grid_attention(q, k, v):
    bsz, seq, n_heads, hd = q.shape
    n_kv = k.shape[2]
    grp = n_heads // n_kv
    nb = seq // BLOCK
    q_blocks = jnp.moveaxis(q.reshape(bsz, nb, BLOCK, n_kv, grp, hd), 1, 0)

    def one_block(qi):
        s = jnp.einsum('bqkgd,bskd->bkgqs', qi, k).astype(jnp.float32) * (hd ** -0.5)
        p = jax.nn.softmax(s, axis=-1)
        return jnp.einsum('bkgqs,bskd->bqkgd', p.astype(v.dtype), v)

    o = lax.map(one_block, q_blocks)
    return jnp.moveaxis(o, 0, 1).reshape(bsz, seq, n_heads * hd)


def token_mixer(h, w_in, a_sink, b_q_norm, b_k_norm, w_branch_a, w_branch_b, w_out):
    bsz, seq, _ = h.shape
    qa, ka, va, qg, kg, vg, ga, gb = jnp.split(h @ w_in, IN_OFFSETS, axis=-1)

    def heads(t):
        return t.reshape(bsz, seq, -1, HEAD_DIM)

    out_a = window_attention(heads(qa), heads(ka), heads(va), a_sink) @ w_branch_a
    rows = seq // GRID_W
    t = jnp.arange(rows * GRID_W)
    row = t // GRID_W
    col = t % GRID_W
    q_b = axial_rope(rms_norm(heads(qg), b_q_norm), row, col)
    k_b = axial_rope(rms_norm(heads(kg), b_k_norm), row, col)
    out_b = grid_attention(q_b, k_b, heads(vg)) @ w_branch_b
    merged = jax.nn.sigmoid(ga) * out_a + jax.nn.sigmoid(gb) * out_b
    return merged @ w_out


def routed_experts(t, w_router, b_router, w_gate, b_gate, w_up, b_up, w_down, b_down):
    n_tok, d = t.shape
    logits = (t @ w_router + b_router).astype(jnp.float32)
    top_v, top_i = lax.top_k(logits, TOP_K)
    top_w = jax.nn.softmax(top_v, axis=-1)
    n_slots = n_tok * TOP_K
    flat_e = top_i.reshape(-1)
    flat_tok = jnp.arange(n_slots) // TOP_K
    flat_w = top_w.reshape(-1)
    order = jnp.argsort(flat_e)
    e_sorted = flat_e[order]
    counts = jnp.bincount(flat_e, length=N_EXPERTS)
    padded = (counts + MOE_BLOCK - 1) // MOE_BLOCK * MOE_BLOCK
    start = jnp.cumsum(counts) - counts
    pad_end = jnp.cumsum(padded)
    pad_start = pad_end - padded
    dest = pad_start[e_sorted] + (jnp.arange(n_slots) - start[e_sorted])
    n_rows = n_slots + N_EXPERTS * MOE_BLOCK
    n_blocks = n_rows // MOE_BLOCK
    row_tok = jnp.zeros((n_rows,), jnp.int32).at[dest].set(flat_tok[order].astype(jnp.int32))
    row_w = jnp.zeros((n_rows,), jnp.float32).at[dest].set(flat_w[order])
    block_e = jnp.minimum(jnp.searchsorted(pad_end, jnp.arange(n_blocks) * MOE_BLOCK, side='right'), N_EXPERTS - 1)
    xs = t[row_tok].reshape(n_blocks, MOE_BLOCK, d)

    def expert_block(args):
        xb, e = args
        g = xb @ w_gate[e] + b_gate[e]
        u = xb @ w_up[e] + b_up[e]
        g = jnp.minimum(g, SWIGLU_LIMIT)
        u = jnp.clip(u, -SWIGLU_LIMIT, SWIGLU_LIMIT)
        act = g * jax.nn.sigmoid(SWIGLU_ALPHA * g) * (u + 1.0)
        return act @ w_down[e] + b_down[e]

    ys = lax.map(expert_block, (xs, block_e)).reshape(n_rows, d)
    out = jax.ops.segment_sum(ys.astype(jnp.float32) * row_w[:, None], row_tok, num_segments=n_tok)
    return out.astype(t.dtype)


def setup_inputs(seed: int = 0) -> dict:
    key = jax.random.key(seed)
    ks = jax.random.split(key, 24)

    def nrm(k, shape, scale):
        return jax.random.normal(k, shape, jnp.float32) * scale

    col_scale = jnp.concatenate([
        jnp.ones((A_Q + A_KV,), jnp.float32), jnp.full((A_KV,), DN_BETA, jnp.float32),
        jnp.ones((B_Q + B_KV,), jnp.float32), jnp.full((B_KV,), DN_BETA, jnp.float32),
        jnp.ones((2 * D_MODEL,), jnp.float32)])
    return {
        "x": nrm(ks[0], (BATCH, SEQ, D_MODEL), 1.0),
        "ln0_g": 1.0 + nrm(ks[1], (D_MODEL,), 0.02),
        "ln0_b": nrm(ks[2], (D_MODEL,), 0.02),
        "w_in": nrm(ks[3], (DEPTH, D_MODEL, IN_WIDTH), D_MODEL ** -0.5) * col_scale,
        "a_sink": nrm(ks[4], (DEPTH, A_HEADS), 0.5),
        "b_q_norm": 1.0 + nrm(ks[5], (DEPTH, HEAD_DIM), 0.02),
        "b_k_norm": 1.0 + nrm(ks[6], (DEPTH, HEAD_DIM), 0.02),
        "w_branch_a": nrm(ks[7], (DEPTH, A_Q, D_MODEL), A_Q ** -0.5),
        "w_branch_b": nrm(ks[8], (DEPTH, B_Q, D_MODEL), B_Q ** -0.5),
        "w_out": nrm(ks[9], (DEPTH, D_MODEL, D_MODEL), D_MODEL ** -0.5 * DN_BETA),
        "ln1_g": 1.0 + nrm(ks[10], (DEPTH, D_MODEL), 0.02),
        "ln1_b": nrm(ks[11], (DEPTH, D_MODEL), 0.02),
        "w_router": nrm(ks[12], (DEPTH, D_MODEL, N_EXPERTS), D_MODEL ** -0.5),
        "b_router": nrm(ks[13], (DEPTH, N_EXPERTS), 0.01),
        "w_gate": nrm(ks[14], (DEPTH, N_EXPERTS, D_MODEL, D_FF), D_MODEL ** -0.5),
        "b_gate": nrm(ks[15], (DEPTH, N_EXPERTS, D_FF), 0.01),
        "w_up": nrm(ks[16], (DEPTH, N_EXPERTS, D_MODEL, D_FF), D_MODEL ** -0.5 * DN_BETA),
        "b_up": nrm(ks[17], (DEPTH, N_EXPERTS, D_FF), 0.01),
        "w_down": nrm(ks[18], (DEPTH, N_EXPERTS, D_FF, D_MODEL), D_FF ** -0.5 * DN_BETA),
        "b_down": nrm(ks[19], (DEPTH, N_EXPERTS, D_MODEL), 0.01),
        "ln2_g": 1.0 + nrm(ks[20], (DEPTH, D_MODEL), 0.02),
        "ln2_b": nrm(ks[21], (DEPTH, D_MODEL), 0.02),
    }


def reference(x, ln0_g, ln0_b, w_in, a_sink, b_q_norm, b_k_norm, w_branch_a, w_branch_b, w_out,
              ln1_g, ln1_b, w_router, b_router, w_gate, b_gate, w_up, b_up, w_down, b_down,
              ln2_g, ln2_b):
    h = layer_norm(x, ln0_g, ln0_b)
    bsz, seq, d = h.shape
    for l in range(DEPTH):
        mix = token_mixer(h, w_in[l], a_sink[l], b_q_norm[l], b_k_norm[l],
                          w_branch_a[l], w_branch_b[l], w_out[l])
        h = layer_norm(DN_ALPHA * h + mix, ln1_g[l], ln1_b[l])
        ffn = routed_experts(h.reshape(bsz * seq, d), w_router[l], b_router[l], w_gate[l], b_gate[l],
                             w_up[l], b_up[l], w_down[l], b_down[l]).reshape(bsz, seq, d)
        h = layer_norm(DN_ALPHA * h + ffn, ln2_g[l], ln2_b[l])
    return h
```

```python
import contextlib
import os
import numpy as np
import concourse.bass as bass
import concourse.mybir as mybir
from concourse.bass_utils import run_bass_kernel_spmd

F32 = mybir.dt.float32
F32R = mybir.dt.float32r
U32 = mybir.dt.uint32
I32 = mybir.dt.int32
ALU = mybir.AluOpType
AF = mybir.ActivationFunctionType
AX = mybir.AxisListType

S = 2048
D = 1024
NCH = 16
NG = 4
NE = 32
CAP = 384
NSLOT = NE * CAP
ALPHA = 2.0 ** 0.25
LN_EPS = 1e-5
RMS_EPS = 1e-6
MASKV = -30000.0
ENGS = ["pe", "act", "dve", "pool", "sp"]


class Op:
    __slots__ = ("eng", "fn", "deps", "signal", "sig_idx", "is_dma", "dma_sem", "dma_val")

    def __init__(self, eng, fn, is_dma):
        self.eng = eng
        self.fn = fn
        self.deps = []
        self.signal = False
        self.sig_idx = None
        self.is_dma = is_dma
        self.dma_sem = None
        self.dma_val = None


class Prog:
    def __init__(self, nc, dma_ring=10):
        self.nc = nc
        self.ops = {e: [] for e in ENGS}
        self.tok = {}
        self.dma_ring = dma_ring
        self.dma_count = {e: 0 for e in ENGS}
        self.dma_ops = {e: [] for e in ENGS}

    def _track(self, o, reads, writes):
        for t in reads:
            st = self.tok.get(t)
            if st is None:
                st = self.tok[t] = [None, []]
            if st[0] is not None:
                o.deps.append((st[0], "raw"))
            st[1].append(o)
        for t in writes:
            st = self.tok.get(t)
            if st is None:
                st = self.tok[t] = [None, []]
            if st[0] is not None:
                o.deps.append((st[0], "waw"))
            for r in st[1]:
                if r is not o:
                    o.deps.append((r, "war"))
            st[0] = o
            st[1] = []

    def op(self, eng, fn, reads=(), writes=()):
        o = Op(eng, fn, False)
        self._track(o, reads, writes)
        self.ops[eng].append(o)
        return o

    def dma(self, eng, fn, reads=(), writes=()):
        o = Op(eng, fn, True)
        self._track(o, reads, writes)
        i = self.dma_count[eng]
        self.dma_count[eng] += 1
        o.dma_sem = (eng, i % self.dma_ring)
        o.dma_val = 16 * (i // self.dma_ring + 1)
        if i >= self.dma_ring:
            o.deps.append((self.dma_ops[eng][i - self.dma_ring], "ring"))
        self.dma_ops[eng].append(o)
        self.ops[eng].append(o)
        return o

    def barrier(self):
        lasts = []
        for e in ENGS:
            comp = [o for o in self.ops[e] if not o.is_dma and o.fn is not None]
            if comp:
                lasts.append(comp[-1])
            lasts.extend(self.dma_ops[e][-self.dma_ring:])
        for e in ENGS:
            o = Op(e, None, False)
            for l in lasts:
                o.deps.append((l, "bar"))
            self.ops[e].append(o)
        self.tok = {}

    @staticmethod
    def _skip(o, d, kind):
        if d.is_dma:
            return False
        if d.eng == o.eng and not o.is_dma and kind != "bar":
            if d.eng == "pe" or kind != "raw":
                return True
        return False

    def emit(self, final_wait):
        nc = self.nc
        for e in ENGS:
            for o in self.ops[e]:
                for d, kind in o.deps:
                    if d.is_dma or self._skip(o, d, kind):
                        continue
                    d.signal = True
        for e in ENGS:
            c = 0
            for o in self.ops[e]:
                if o.signal:
                    c += 1
                    o.sig_idx = c
        with contextlib.ExitStack() as st:
            st.enter_context(nc.cleanup_on_exit())
            esem = {e: nc.alloc_semaphore(name="s_" + e) for e in ENGS}
            dsem = {}
            for e in ENGS:
                for i in range(min(self.dma_ring, self.dma_count[e])):
                    dsem[(e, i)] = nc.alloc_semaphore(name="d_%s_%d" % (e, i))
            for sm in list(esem.values()) + list(dsem.values()):
                nc.gpsimd.sem_clear(sm)
            block = st.enter_context(nc.Block())

            def run(e, eng):
                seen = {}
                for o in self.ops[e]:
                    need = {}
                    for d, kind in o.deps:
                        if self._skip(o, d, kind):
                            continue
                        if d.is_dma:
                            key, sem, val = ("d",) + d.dma_sem, dsem[d.dma_sem], d.dma_val
                        else:
                            key, sem, val = ("e", d.eng), esem[d.eng], d.sig_idx
                        if seen.get(key, 0) >= val:
                            continue
                        if key not in need or need[key][1] < val:
                            need[key] = (sem, val)
                    for key, (sem, val) in need.items():
                        eng.wait_ge(sem, val)
                        seen[key] = val
                    if o.fn is None:
                        continue
                    ins = o.fn(eng)
                    if o.is_dma:
                        ins.then_inc(dsem[o.dma_sem], 16)
                    elif o.signal:
                        ins.then_inc(esem[e], 1)
                for d in final_wait.get(e, []):
                    eng.wait_ge(dsem[d.dma_sem], d.dma_val)

            block.tensor(lambda eng: run("pe", eng))
            block.scalar(lambda eng: run("act", eng))
            block.vector(lambda eng: run("dve", eng))
            block.gpsimd(lambda eng: run("pool", eng))
            block.sync(lambda eng: run("sp", eng))


def _consts():
    c = {}
    c["ident"] = np.eye(128, dtype=np.float32)
    bo = np.zeros((128, 128), np.float32)
    bo[:64, :64] = 1.0 / 64
    bo[64:, 64:] = 1.0 / 64
    c["blockones"] = bo
    rot = np.zeros((128, 128), np.float32)
    for m in range(128):
        r = (m % 64) % 32
        if r < 16:
            rot[m + 16, m] = -1.0
        else:
            rot[m - 16, m] = 1.0
    c["rot"] = rot
    t = np.arange(S)
    row = (t // 64).astype(np.float32)
    col = (t % 64).astype(np.float32)
    inv = (np.float32(10000.0) ** (-np.arange(16, dtype=np.float32) * np.float32(2.0 / 32))).astype(np.float32)
    cos = np.zeros((128, S), np.float32)
    sin = np.zeros((128, S), np.float32)
    for p in range(128):
        f = p % 64
        pos = row if f < 32 else col
        ang = (pos * inv[f % 16]).astype(np.float32)
        cos[p] = np.cos(ang)
        sin[p] = np.sin(ang)
    c["rcos"] = cos
    c["rsin"] = sin
    slopes = np.array([2.0 ** (-8.0 * (h + 1) / 8) for h in range(8)], np.float32)
    bias = np.zeros((128, 3, 2, 4, 128), np.float32)
    ki = np.arange(128)[:, None]
    qi = np.arange(128)[None, :]
    for oi, o in enumerate((-1, 0, 1)):
        dist = np.abs(qi - ki - 128 * o)
        for kv in range(2):
            for j in range(4):
                h = j + 4 * kv
                bias[:, oi, kv, j, :] = np.where(dist <= 128, -slopes[h] * dist.astype(np.float32), MASKV)
    c["abias"] = bias.reshape(128, 6 * 512)
    tri = (np.arange(128)[:, None] < np.arange(128)[None, :]).astype(np.float32)
    c["tri"] = tri
    c["iota32"] = np.tile(np.arange(32, dtype=np.float32)[None, :], (128, 1))
    return c


def build_nc(stage=3, stop=None):
    nc = bass.Bass("TRN2", target_bir_lowering=False)
    nc.dge_precook = False

    def din(name, shape, dt=F32):
        return nc.dram_tensor(name, list(shape), dt, kind="ExternalInput").ap()

    x_d = din("x", [S, D])
    lnp_d = din("lnp", [6, D])
    win_d = din("w_in", [D, 3584], F32R)
    small_d = din("small", [1, 8 + 64 + 64 + 32])
    wba_d = din("w_ba", [512, D], F32R)
    wbb_d = din("w_bb", [512, D], F32R)
    wout_d = din("w_out", [D, D], F32R)
    wr_d = din("w_router", [D, NE])
    wg_d = din("w_gate", [NE, D, D], F32R)
    wu_d = din("w_up", [NE, D, D], F32R)
    wd_d = din("w_down", [NE, D, D], F32R)
    bg_d = din("b_gate", [NE, D])
    bu_d = din("b_up", [NE, D])
    bd_d = din("b_down", [NE, D])
    c_ident = din("ident", [128, 128])
    c_bo = din("blockones", [128, 128], F32R)
    c_rot = din("rot", [128, 128], F32R)
    c_cos = din("rcos", [128, S])
    c_sin = din("rsin", [128, S])
    c_abias = din("abias", [128, 3072])
    c_tri = din("tri", [128, 128])
    c_iota = din("iota32", [128, 32])
    out_d = nc.dram_tensor("out", [S, D], F32, kind="ExternalOutput").ap()
    dbg_d = None
    if stage < 3:
        dbg_d = nc.dram_tensor("dbg", [S, D], F32, kind="ExternalOutput").ap()
    H0_d = nc.dram_tensor("H0", [S, D], F32, kind="Internal").ap()
    H0T_d = nc.dram_tensor("H0T", [NG, 128, 8 * 512], F32R, kind="Internal").ap()
    H1_d = nc.dram_tensor("H1", [S, D], F32, kind="Internal").ap()
    XS_d = nc.dram_tensor("XS", [NSLOT, D], F32, kind="Internal").ap()
    YS_d = nc.dram_tensor("YS", [NSLOT, D], F32, kind="Internal").ap()

    with contextlib.ExitStack() as st:
        def sb(name, shape, dt=F32):
            return st.enter_context(nc.sbuf_tensor(name, list(shape), dt))

        def psb(name):
            return st.enter_context(nc.psum_tensor(name, [128, 512], F32))

        UA = sb("UA", [128, 20480], F32R)
        UB = sb("UB", [128, 8320], F32R)
        expb = sb("expb", [128, 3, 512], F32R)
        tmpR = sb("tmpR", [128, 2, 512], F32R)
        tmpO = sb("tmpO", [128, 512], F32R)
        expb2 = sb("expb2", [128, 512], F32R)
        cbo = sb("cbo", [128, 128], F32R)
        crot = sb("crot", [128, 128], F32R)
        UC = sb("UC", [128, 7168], F32)
        wk = sb("wk", [128, 4, 1024], F32)
        lnG = sb("lnG", [128, 1024], F32)
        lnB = sb("lnB", [128, 1024], F32)
        ident = sb("identt", [128, 128], F32)
        tri = sb("trit", [128, 128], F32)
        onesM = sb("onesM", [128, 128], F32)
        iota = sb("iotat", [128, 32], F32)
        smallb = sb("smallb", [128, 168], F32)
        esink = sb("esinkt", [128, 8], F32)
        gqk = sb("gqk", [128, 2], F32)
        stats = sb("stats", [128, 2, 6], F32)
        mv = sb("mvt", [128, 2], F32)
        lnt = sb("lnt", [128, 4], F32)
        rcp = sb("rcp", [128, 512], F32)
        bcs = sb("bcs", [128, 512], F32)
        WdT = sb("WdT", [32, 2048], F32)
        bdn = sb("bdn", [32, 1024], F32)
        bgT = sb("bgT", [128, 8, 32], F32)
        buT = sb("buT", [128, 8, 32], F32)
        wrt = sb("wrt", [128, 8, 32], F32)
        rt = sb("rt", [128, 640], F32)
        rti = sb("rti", [128, 8], U32)
        posi = sb("posi", [128, NCH, 4], I32)
        wts = sb("wts", [128, NCH, 4], F32)
        msum = sb("msum", [128, 32], F32)
        PS = [psb("ps%d" % i) for i in range(8)]

        P = Prog(nc)
        print("sbuf bytes remaining", nc.sbuf_bytes_remaining)

        def MM(out, lhsT, rhs, start, stop, r, w):
            P.op("pe", lambda e: e.matmul(out, lhsT, rhs, start=start, stop=stop), r, w)

        def TR(out, in_, idn, r, w):
            P.op("pe", lambda e: e.transpose(out, in_, idn), list(r) + ["ident"], w)

        def ACTF(out, in_, func, r, w, bias=0.0, scale=1.0, accum=None):
            if accum is None:
                P.op("act", lambda e: e.activation(out, in_, func, bias=bias, scale=scale), r, w)
            else:
                P.op("act", lambda e: e.activation(out, in_, func, bias=bias, scale=scale, accum_out=accum), r, w)

        def TT(eng, out, a, b, op, r, w):
            P.op(eng, lambda e: e.tensor_tensor(out, a, b, op), r, w)

        def TS(eng, out, a, s1, s2, op0, op1, r, w):
            if s2 is None:
                P.op(eng, lambda e: e.tensor_scalar(out, a, s1, None, op0), r, w)
            else:
                P.op(eng, lambda e: e.tensor_scalar(out, a, s1, s2, op0, op1), r, w)

        def STT(eng, out, in0, scalar, in1, op0, op1, r, w):
            P.op(eng, lambda e: e.scalar_tensor_tensor(out, in0, scalar, in1, op0, op1), r, w)

        def CP(eng, out, in_, r, w):
            if eng == "act":
                P.op("act", lambda e: e.copy(out, in_), r, w)
            else:
                P.op(eng, lambda e: e.tensor_copy(out, in_), r, w)

        def DMA(q, out, in_, r, w):
            return P.dma(q, lambda e: e.dma_start(out=out, in_=in_), r, w)

        def layer_norm(src, dst, tmp, r, w, tag):
            P.op("dve", lambda e: e.bn_stats(stats[:, 0, :], src[:, 0:512]), r, ["st0"])
            P.op("dve", lambda e: e.bn_stats(stats[:, 1, :], src[:, 512:1024]), r, ["st1"])
            P.op("dve", lambda e: e.bn_aggr(mv[:], stats[:]), ["st0", "st1"], ["mv"])
            ACTF(lnt[:, 0:1], mv[:, 1:2], AF.Sqrt, ["mv"], ["ln_sd"], bias=LN_EPS)
            P.op("dve", lambda e: e.reciprocal(lnt[:, 1:2], lnt[:, 0:1]), ["ln_sd"], ["ln_rstd"])
            TS("dve", lnt[:, 2:3], mv[:, 0:1], lnt[:, 1:2], -1.0, ALU.mult, ALU.mult, ["mv", "ln_rstd"], ["ln_nmr"])
            ACTF(tmp, src, AF.Identity, list(r) + ["ln_rstd", "ln_nmr"], [tag + "_t"], bias=lnt[:, 2:3], scale=lnt[:, 1:2])
            TT("pool", tmp, tmp, lnG[:], ALU.mult, [tag + "_t", "lnG"], [tag + "_t2"])
            TT("dve", dst, tmp, lnB[:], ALU.add, [tag + "_t2", "lnB"], w)

        ring = [UA[:, i * 4096:(i + 1) * 4096].rearrange("p (k n) -> p k n", k=8) for i in range(5)]
        h0Tg = UA[:, 8192:12288].rearrange("p (k n) -> p k n", k=8)
        qAT = UA[:, 12288:14336].rearrange("p (j n) -> p j n", j=4)
        qBT = UA[:, 14336:16384].rearrange("p (j n) -> p j n", j=4)
        OAT = UA[:, 16384:18432].rearrange("p (j n) -> p j n", j=4)
        OBT = UA[:, 18432:20480].rearrange("p (j n) -> p j n", j=4)
        mrg = UA[:, 12288:16384].rearrange("p (k n) -> p k n", k=8)
        kAT = UB[:, 0:2048]
        kBT = UB[:, 2048:4096]
        vAa = UB[:, 4096:6208].rearrange("p (c g d) -> p c g d", c=16, g=2)
        vBa = UB[:, 6208:8320].rearrange("p (c g d) -> p c g d", c=16, g=2)
        xTe = UB[:, 0:3072].rearrange("p (k n) -> p k n", k=8)
        actT = UB[:, 3072:6144].rearrange("p (k n) -> p k n", k=8)
        rcosg = UC[:, 0:512]
        rsing = UC[:, 512:1024]
        abias = UC[:, 1024:4096].rearrange("p (o n) -> p o n", o=6)
        t_rstd = UC[:, 4096:4608]
        t_a = UC[:, 4608:5120]
        t_b = UC[:, 5120:5632]
        t_sig = UC[:, 5632:6144]
        t_sp0 = UC[:, 6144:6656]
        t_sp1 = UC[:, 6656:7168]
        Xe = UC[:, 0:3072].rearrange("p (j d) -> p j d", j=3)
        gT = UC[:, 3072:3456]
        sgT = UC[:, 3456:3840]
        uT = UC[:, 3840:4224]
        t1T = UC[:, 4224:4608]
        ytile = [UC[:, 4608:5632], UC[:, 5632:6656]]

        DMA("sp", ident[:], c_ident, [], ["ident"])
        DMA("sp", cbo[:], c_bo, [], ["cbo"])
        DMA("sp", crot[:], c_rot, [], ["crot"])
        DMA("sp", tri[:], c_tri, [], ["tri"])
        DMA("sp", iota[:], c_iota, [], ["iota"])
        DMA("sp", smallb[:], small_d.to_broadcast([128, 168]), [], ["smallb"])
        DMA("sp", abias, c_abias.rearrange("p (o n) -> p o n", o=6), [], ["abias"])
        DMA("sp", lnG[:], lnp_d[0:1, :].to_broadcast([128, D]), [], ["lnG"])
        DMA("sp", lnB[:], lnp_d[1:2, :].to_broadcast([128, D]), [], ["lnB"])
        P.op("pool", lambda e: e.memset(onesM[:], 1.0), [], ["onesM"])
        ACTF(esink[:], smallb[:, 0:8], AF.Exp, ["smallb"], ["esink"])
        gsel = rt[:, 0:64]
        TT("dve", gsel, smallb[:, 8:72], ident[:, 0:64], ALU.mult, ["smallb", "ident"], ["gsel"])
        TT("dve", rt[:, 64:128], smallb[:, 8:72], ident[:, 64:128], ALU.mult, ["smallb", "ident"], ["gsel2"])
        TT("dve", gsel, gsel, rt[:, 64:128], ALU.add, ["gsel", "gsel2"], ["gsel3"])
        P.op("dve", lambda e: e.reduce_sum(gqk[:, 0:1], gsel, AX.X), ["gsel3"], ["gq"])
        gsel_k = rt[:, 128:192]
        TT("dve", gsel_k, smallb[:, 72:136], ident[:, 0:64], ALU.mult, ["smallb", "ident"], ["gselk"])
        TT("dve", rt[:, 192:256], smallb[:, 72:136], ident[:, 64:128], ALU.mult, ["smallb", "ident"], ["gselk2"])
        TT("dve", gsel_k, gsel_k, rt[:, 192:256], ALU.add, ["gselk", "gselk2"], ["gselk3"])
        P.op("dve", lambda e: e.reduce_sum(gqk[:, 1:2], gsel_k, AX.X), ["gselk3"], ["gk"])
        ones_v = onesM[:, 0:32].rearrange("p (c g d) -> p c g d", c=16, g=2)
        CP("dve", vAa[:, :, :, 64:65], ones_v, ["onesM"], ["vA_ones"])
        CP("dve", vBa[:, :, :, 64:65], ones_v, ["onesM"], ["vB_ones"])

        def rms_rope(ps_in, ps_tok, gcol, dst, dst_tok, bset=0):
            if bset == 0:
                sq, sqk, kn, knk = tmpR[:, 0, :], "tmpR0", tmpR[:, 1, :], "tmpR1"
                rs, rsk, ta, tak, tb, tbk = t_rstd, "t_rstd", t_a, "t_a", t_b, "t_b"
                p6, p7 = 6, 7
            else:
                sq, sqk, kn, knk = expb[:, 0, :], ("expb", 0), expb[:, 1, :], ("expb", 1)
                rs, rsk, ta, tak, tb, tbk = t_sig, "t_sig", t_sp0, "t_sp0", t_sp1, "t_sp1"
                p6, p7 = 2, 3
            ACTF(sq, ps_in, AF.Square, [ps_tok], [sqk])
            MM(PS[p6][:], cbo[:], sq, True, True, ["cbo", sqk], [("ps", p6)])
            ACTF(rs, PS[p6][:], AF.Sqrt, [("ps", p6)], [rsk], bias=RMS_EPS)
            P.op("dve", lambda e: e.reciprocal(rs, rs), [rsk], [rsk])
            STT("dve", kn, ps_in, gcol, rs, ALU.mult, ALU.mult, [ps_tok, rsk, "gq", "gk"], [knk])
            MM(PS[p7][:], crot[:], kn, True, True, ["crot", knk], [("ps", p7)])
            TT("pool", ta, kn.bitcast(F32), rcosg, ALU.mult, [knk, "rope"], [tak])
            TT("dve", tb, PS[p7][:], rsing, ALU.mult, [("ps", p7), "rope"], [tbk])
            TT("dve", dst, ta, tb, ALU.add, [tak, tbk], dst_tok)

        kv_unit = ring[0]
        p1_groups = NG if stop not in ("c0", "ln", "tr", "k", "kr", "v") else (0 if stop == "c0" else 1)
        DMA("sp", kv_unit, win_d[:, 1024:1536].rearrange("(k p) n -> p k n", p=128), [], [("ring", 0)])
        for g in range(p1_groups):
            DMA("sp", rcosg, c_cos[:, g * 512:(g + 1) * 512], [], ["rope"])
            DMA("sp", rsing, c_sin[:, g * 512:(g + 1) * 512], [], ["rope"])
            for j in range(4):
                c = g * 4 + j
                xt = wk[:, j, :]
                DMA("sp", xt, x_d[c * 128:(c + 1) * 128, :], [], [("wk", j)])
                layer_norm(xt, xt, xt, [("wk", j)], [("wk", j)], "ln0")
                DMA("act", H0_d[c * 128:(c + 1) * 128, :], xt, [("wk", j)], [("H0", c)])
                if stop == "ln":
                    continue
                for half in range(2):
                    pst = PS[2 + half]
                    for kk in range(4):
                        k = half * 4 + kk
                        TR(pst[:, kk * 128:(kk + 1) * 128], xt[:, k * 128:(k + 1) * 128], ident[:], [("wk", j)], [("ps", 2 + half)])
                    CP("act" if half == 0 else "dve",
                       h0Tg[:, half * 4:(half + 1) * 4, j * 128:(j + 1) * 128],
                       pst[:].rearrange("p (k n) -> p k n", k=4), [("ps", 2 + half)], ["h0Tg"])
            if stop == "ln":
                continue
            DMA("act", H0T_d[g], UA[:, 8192:12288], ["h0Tg"], [("H0T", g)])
            if stop == "tr":
                continue
            for blk in range(2):
                psk = PS[blk]
                for k in range(8):
                    MM(psk[:], kv_unit[:, k, blk * 128:(blk + 1) * 128], h0Tg[:, k, :], k == 0, k == 7,
                       [("ring", 0), "h0Tg"], [("ps", blk)])
            CP("act", kAT[:, g * 512:(g + 1) * 512], PS[0][:], [("ps", 0)], ["kAT"])
            if stop == "k":
                continue
            rms_rope(PS[1][:], ("ps", 1), gqk[:, 1:2], kBT[:, g * 512:(g + 1) * 512], ["kBT"])
            if stop == "kr":
                continue
            for j in range(4):
                c = g * 4 + j
                psv = PS[4 + (j % 2)]
                for k in range(8):
                    MM(psv[:, 0:256], h0Tg[:, k, j * 128:(j + 1) * 128], kv_unit[:, k, 256:512], k == 0, k == 7,
                       [("ring", 0), "h0Tg"], [("ps", 4 + (j % 2))])
                veng = "act" if j % 2 == 0 else "dve"
                CP(veng, vAa[:, c, :, 0:64], psv[:, 0:128].rearrange("p (g d) -> p g d", g=2),
                   [("ps", 4 + (j % 2))], ["vA"])
                CP(veng, vBa[:, c, :, 0:64], psv[:, 128:256].rearrange("p (g d) -> p g d", g=2),
                   [("ps", 4 + (j % 2))], ["vB"])

        DMA("sp", lnG[:], lnp_d[2:3, :].to_broadcast([128, D]), [], ["lnG"])
        DMA("sp", lnB[:], lnp_d[3:4, :].to_broadcast([128, D]), [], ["lnB"])
        rstate = {"n": 0}

        def load_unit(src_ap, kch=8):
            i = rstate["n"] % 2
            rstate["n"] += 1
            DMA("sp", ring[i][:, 0:kch, :], src_ap, [], [("ring", i)])
            return ring[i], ("ring", i)

        def wunit(w_ap, c0, kchunks=8):
            return w_ap[:, c0:c0 + 512].rearrange("(k p) n -> p k n", p=128)

        def phase2():
            for g in range(NG):
                DMA("sp", UA[:, 8192:12288], H0T_d[g], [("H0T", g)], ["h0Tg"])
                DMA("sp", rcosg, c_cos[:, g * 512:(g + 1) * 512], [], ["rope"])
                DMA("sp", rsing, c_sin[:, g * 512:(g + 1) * 512], [], ["rope"])
                u, ut = load_unit(wunit(win_d, 0))
                for j in range(4):
                    psq = PS[j % 2]
                    for k in range(8):
                        MM(psq[:], u[:, k, j * 128:(j + 1) * 128], h0Tg[:, k, :], k == 0, k == 7, [ut, "h0Tg"], [("ps", j % 2)])
                    CP("act", qAT[:, j, :], psq[:], [("ps", j % 2)], ["qAT", ("mrg", j)])
                u, ut = load_unit(wunit(win_d, 512))
                for j in range(4):
                    psq = PS[j % 2]
                    for k in range(8):
                        MM(psq[:], u[:, k, j * 128:(j + 1) * 128], h0Tg[:, k, :], k == 0, k == 7, [ut, "h0Tg"], [("ps", j % 2)])
                    rms_rope(psq[:], ("ps", j % 2), gqk[:, 0:1], qBT[:, j, :], ["qBT", ("mrg", 4 + j)], bset=j % 2)

                if stop == 'q':
                    return
                bufsets = [
                    dict(banks=[2, 3, 4], ex=[expb[:, 0, :], expb[:, 1, :], expb[:, 2, :]],
                         extok=[("expb", 0), ("expb", 1), ("expb", 2)],
                         ts=[t_a, t_b, t_sig], tstok=["t_a", "t_b", "t_sig"]),
                    dict(banks=[0, 1, 6], ex=[tmpR[:, 0, :], tmpR[:, 1, :], expb2[:]],
                         extok=["tmpR0", "tmpR1", ("expb", 5)],
                         ts=[t_rstd, t_sp0, t_sp1], tstok=["t_rstd", "t_sp0", "t_sp1"]),
                ]

                def a_stage_x(nq, kv, bs):
                    n = g * 4 + nq
                    offs = [o for o in (-1, 0, 1) if 0 <= n + o < NCH]
                    pb = slice(kv * 64, (kv + 1) * 64)
                    for oi, o in enumerate(offs):
                        c = n + o
                        bk = bs["banks"][oi]
                        pss = PS[bk]
                        for j in range(4):
                            MM(pss[:, j * 128:(j + 1) * 128], kAT[pb, c * 128:(c + 1) * 128],
                               qAT[pb, j, nq * 128:(nq + 1) * 128], True, True, ["kAT", "qAT"], [("ps", bk)])
                        STT("dve", bs["ts"][oi], pss[:], 0.125, abias[:, (o + 1) * 2 + kv, :], ALU.mult, ALU.add,
                            [("ps", bk), "abias"], [bs["tstok"][oi]])
                        ACTF(bs["ex"][oi], bs["ts"][oi], AF.Exp, [bs["tstok"][oi]], [bs["extok"][oi]])

                def a_stage_y(nq, kv, bs):
                    n = g * 4 + nq
                    offs = [o for o in (-1, 0, 1) if 0 <= n + o < NCH]
                    pso = PS[5]
                    for j in range(4):
                        for oi, o in enumerate(offs):
                            c = n + o
                            MM(pso[0:65, j * 128:(j + 1) * 128], vAa[:, c, kv, 0:65], bs["ex"][oi][:, j * 128:(j + 1) * 128],
                               oi == 0, oi == len(offs) - 1, ["vA", "vA_ones", bs["extok"][oi]], [("ps", 5)])
                    TT("dve", rcp[64:65, :].rearrange("p (j q) -> p j q", j=4),
                       pso[64:65, :].rearrange("p (j q) -> p j q", j=4),
                       esink[64:65, 4 * kv:4 * kv + 4].unsqueeze(2).to_broadcast([1, 4, 128]), ALU.add,
                       [("ps", 5), "esink"], ["rcp"])
                    P.op("dve", lambda e: e.reciprocal(rcp[64:65, :], rcp[64:65, :]), ["rcp"], ["rcp"])
                    MM(PS[7][0:64, :], onesM[64:65, 0:64], rcp[64:65, :], True, True, ["onesM", "rcp"], [("ps", 7)])
                    CP("dve", bcs[0:64, :], PS[7][0:64, :], [("ps", 7)], ["bcs"])
                    if kv == 0:
                        TT("dve", OAT[0:64, :, nq * 128:(nq + 1) * 128], pso[0:64, :].rearrange("p (j q) -> p j q", j=4),
                           bcs[0:64, :].rearrange("p (j q) -> p j q", j=4), ALU.mult, [("ps", 5), "bcs"], ["OAT"])
                    else:
                        TT("dve", tmpO[0:64, :], pso[0:64, :], bcs[0:64, :], ALU.mult, [("ps", 5), "bcs"], ["tmpO"])
                        P.dma("pool", lambda e, nq=nq: e.dma_start(
                            out=OAT[64:128, :, nq * 128:(nq + 1) * 128],
                            in_=tmpO[0:64, :].rearrange("p (j q) -> p j q", j=4)), ["tmpO"], ["OAT"])

                units = [(nq, kv) for nq in range(4) for kv in range(2)]
                a_stage_x(units[0][0], units[0][1], bufsets[0])
                for ui, (nq, kv) in enumerate(units):
                    if ui + 1 < len(units):
                        a_stage_x(units[ui + 1][0], units[ui + 1][1], bufsets[(ui + 1) % 2])
                    a_stage_y(nq, kv, bufsets[ui % 2])

                if stop == 'A':
                    return
                extB = [(expb[:, 0, :], ("expb", 0)), (expb[:, 1, :], ("expb", 1)), (expb[:, 2, :], ("expb", 2))]
                for kv in range(2):
                    pb = slice(kv * 64, (kv + 1) * 64)
                    for j in range(4):
                        pso = PS[5 + (j % 2)]

                        def b_qk(c, j=j, pb=pb):
                            bk = 2 + (c % 3)
                            MM(PS[bk][:], kBT[pb, c * 128:(c + 1) * 128], qBT[pb, j, :], True, True, ["kBT", "qBT"], [("ps", bk)])
                            ACTF(extB[c % 3][0], PS[bk][:], AF.Exp, [("ps", bk)], [extB[c % 3][1]], scale=0.125)

                        b_qk(0)
                        b_qk(1)
                        for c in range(NCH):
                            if c + 2 < NCH:
                                b_qk(c + 2)
                            MM(pso[0:65, :], vBa[:, c, kv, 0:65], extB[c % 3][0], c == 0, c == NCH - 1,
                               ["vB", "vB_ones", extB[c % 3][1]], [("ps", 5 + (j % 2))])
                        P.op("dve", lambda e, pso=pso: e.reciprocal(rcp[64:65, :], pso[64:65, :]), [("ps", 5 + (j % 2))], ["rcp"])
                        MM(PS[7][0:64, :], onesM[64:65, 0:64], rcp[64:65, :], True, True, ["onesM", "rcp"], [("ps", 7)])
                        CP("dve", bcs[0:64, :], PS[7][0:64, :], [("ps", 7)], ["bcs"])
                        if kv == 0:
                            TT("dve", OBT[0:64, j, :], pso[0:64, :], bcs[0:64, :], ALU.mult, [("ps", 5 + (j % 2)), "bcs"], ["OBT"])
                        else:
                            TT("dve", tmpO[0:64, :], pso[0:64, :], bcs[0:64, :], ALU.mult, [("ps", 5 + (j % 2)), "bcs"], ["tmpO"])
                            P.dma("pool", lambda e, j=j: e.dma_start(out=OBT[64:128, j, :], in_=tmpO[0:64, :]), ["tmpO"], ["OBT"])

                if stop == 'B':
                    return
                u, ut = load_unit(wba_d[:, 0:512].rearrange("(k p) n -> p k n", p=128), 4)
                u2, ut2 = load_unit(wba_d[:, 512:1024].rearrange("(k p) n -> p k n", p=128), 4)
                for m in range(8):
                    uu, uut = (u, ut) if m < 4 else (u2, ut2)
                    psm = PS[m % 2]
                    for k in range(4):
                        MM(psm[:], uu[:, k, (m % 4) * 128:(m % 4 + 1) * 128], OAT[:, k, :], k == 0, k == 3, [uut, "OAT"], [("ps", m % 2)])
                    CP("act", mrg[:, m, :], psm[:], [("ps", m % 2)], [("mrg", m), "qAT" if m < 4 else "qBT"])
                for hf in range(2):
                    u, ut = load_unit(wunit(win_d, 1536 + hf * 512))
                    for mm_ in range(4):
                        m = hf * 4 + mm_
                        psm = PS[m % 2]
                        for k in range(8):
                            MM(psm[:], u[:, k, mm_ * 128:(mm_ + 1) * 128], h0Tg[:, k, :], k == 0, k == 7, [ut, "h0Tg"], [("ps", m % 2)])
                        sgb, sgk = ((t_sig, "t_sig"), (t_sp0, "t_sp0"))[m % 2]
                        ACTF(sgb, psm[:], AF.Sigmoid, [("ps", m % 2)], [sgk])
                        TT("dve", mrg[:, m, :], mrg[:, m, :].bitcast(F32), sgb, ALU.mult, [("mrg", m), sgk], [("mrg", m)])
                for hf in range(2):
                    u, ut = load_unit(wbb_d[:, hf * 512:(hf + 1) * 512].rearrange("(k p) n -> p k n", p=128), 4)
                    u2, ut2 = load_unit(wunit(win_d, 2560 + hf * 512))
                    for mm_ in range(4):
                        m = hf * 4 + mm_
                        pa, pg = (0, 1) if m % 2 == 0 else (2, 3)
                        sgb, sgk = ((t_sig, "t_sig"), (t_sp0, "t_sp0"))[m % 2]
                        tab, tak = ((t_a, "t_a"), (t_b, "t_b"))[m % 2]
                        for k in range(4):
                            MM(PS[pa][:], u[:, k, mm_ * 128:(mm_ + 1) * 128], OBT[:, k, :], k == 0, k == 3, [ut, "OBT"], [("ps", pa)])
                        for k in range(8):
                            MM(PS[pg][:], u2[:, k, mm_ * 128:(mm_ + 1) * 128], h0Tg[:, k, :], k == 0, k == 7, [ut2, "h0Tg"], [("ps", pg)])
                        ACTF(sgb, PS[pg][:], AF.Sigmoid, [("ps", pg)], [sgk])
                        TT("dve", tab, PS[pa][:], sgb, ALU.mult, [("ps", pa), sgk], [tak])
                        TT("dve", mrg[:, m, :], mrg[:, m, :].bitcast(F32), tab, ALU.add, [("mrg", m), tak], [("mrg", m)])
                if stop == 'M':
                    return
                uo = []
                for hf in range(2):
                    uo.append(load_unit(wunit(wout_d, hf * 512)))
                mtoks = [("mrg", m) for m in range(8)]
                for j in range(4):
                    c = g * 4 + j
                    h0t = wk[:, j, :]
                    DMA("sp", h0t, H0_d[c * 128:(c + 1) * 128, :], [("H0", c)], [("wk", j)])
                    for hf in range(2):
                        u, ut = uo[hf]
                        psw = PS[hf]
                        for k in range(8):
                            MM(psw[:], mrg[:, k, j * 128:(j + 1) * 128], u[:, k, :], k == 0, k == 7, [ut] + mtoks, [("ps", hf)])
                        STT("dve", h0t[:, hf * 512:(hf + 1) * 512], h0t[:, hf * 512:(hf + 1) * 512], ALPHA, psw[:],
                            ALU.mult, ALU.add, [("wk", j), ("ps", hf)], [("wk", j)])
                    layer_norm(h0t, h0t, h0t, [("wk", j)], [("wk", j)], "ln1")
                    DMA("act", H1_d[c * 128:(c + 1) * 128, :], h0t, [("wk", j)], [("H1", c)])
                    if stage == 1:
                        DMA("act", dbg_d[c * 128:(c + 1) * 128, :], h0t, [("wk", j)], [("dbg", c)])


        if stop not in ("p1", "c0", "ln", "tr", "k", "kr", "v"):
            phase2()
        outs = []
        if stage >= 2:
            P.barrier()
            DMA("sp", wrt[:], wr_d.rearrange("(k p) n -> p k n", p=128), [], ["wrt"])
            DMA("sp", bdn[:], bd_d, [], ["bdn"])
            bgs = wk[0:32, 2, :]
            bus = wk[0:32, 3, :]
            DMA("sp", bgs, bg_d, [], ["bgs"])
            DMA("sp", bus, bu_d, [], ["bus"])
            for (src, stok, dstT, dtok) in ((bgs, "bgs", bgT, "bgT"), (bus, "bus", buT, "buT")):
                pst = PS[0]
                for m in range(8):
                    TR(pst[:, m * 32:(m + 1) * 32], src[:, m * 128:(m + 1) * 128], ident[0:32, 0:32], [stok], [("ps", 0)])
                CP("dve", dstT[:], pst[:, 0:256].rearrange("p (m e) -> p m e", m=8), [("ps", 0)], [dtok])
            P.op("pool", lambda e: e.memset(msum[:], 0.0), [], ["msum"])
            brt = smallb[:, 136:168]
            for c in range(NCH):
                h1t = wk[:, c % 2, :]
                htok = ("wk", c % 2)
                DMA("sp", h1t, H1_d[c * 128:(c + 1) * 128, :], [], [htok])
                for half in range(2):
                    pst = PS[2 + half]
                    for kk in range(4):
                        k = half * 4 + kk
                        TR(pst[:, kk * 128:(kk + 1) * 128], h1t[:, k * 128:(k + 1) * 128], ident[:], [htok], [("ps", 2 + half)])
                    CP("act" if half == 0 else "dve", (rcp if half == 0 else bcs)[:], pst[:], [("ps", 2 + half)],
                       ["h1T%d" % half])
                psl = PS[4]
                for k in range(8):
                    srcT = (rcp if k < 4 else bcs)[:, (k % 4) * 128:(k % 4 + 1) * 128]
                    MM(psl[:, 0:32], srcT, wrt[:, k, :], k == 0, k == 7, ["h1T0", "h1T1", "wrt"], [("ps", 4)])
                lg = rt[:, 0:32]
                TT("dve", lg, psl[:, 0:32], brt, ALU.add, [("ps", 4), "smallb"], ["lg"])
                mx = rt[:, 32:40]
                P.op("dve", lambda e, mx=mx, lg=lg: e.max(mx, lg), ["lg"], ["mx"])
                P.op("dve", lambda e, mx=mx, lg=lg: e.max_index(rti[:], mx, lg), ["lg", "mx"], ["rti"])
                negm = rt[:, 40:41]
                TS("dve", negm, mx[:, 0:1], -1.0, None, ALU.mult, None, ["mx"], ["negm"])
                e4 = rt[:, 44:48]
                esum = rt[:, 48:49]
                ACTF(e4, mx[:, 0:4], AF.Exp, ["mx", "negm"], ["e4", "esum"], bias=negm, accum=esum)
                P.op("dve", lambda e, esum=esum: e.reciprocal(rt[:, 49:50], esum), ["esum"], ["ersum"])
                TS("dve", wts[:, c, :], e4, rt[:, 49:50], None, ALU.mult, None, ["e4", "ersum"], ["wts"])
                mk = rt[:, 64:96]
                TS("dve", mk, lg, mx[:, 3:4], None, ALU.is_ge, None, ["lg", "mx"], ["mk"])
                psr = PS[5]
                if c > 0:
                    MM(psr[:, 0:32], onesM[:], msum[:], True, False, ["onesM", "msum"], [("ps", 5)])
                MM(psr[:, 0:32], tri[:], mk, c == 0, True, ["tri", "mk"], [("ps", 5)])
                TT("pool", msum[:], msum[:], mk, ALU.add, ["msum", "mk"], ["msum"])
                idxf = rt[:, 96:100]
                CP("dve", idxf, rti[:, 0:4], ["rti"], ["idxf"])
                oh = rt[:, 128:256].rearrange("p (k e) -> p k e", k=4)
                TT("dve", oh, iota[:].unsqueeze(1).to_broadcast([128, 4, 32]),
                   idxf.unsqueeze(2).to_broadcast([128, 4, 32]), ALU.is_equal, ["iota", "idxf"], ["oh"])
                pr = rt[:, 256:384].rearrange("p (k e) -> p k e", k=4)
                TT("dve", pr, oh, psr[:, 0:32].unsqueeze(1).to_broadcast([128, 4, 32]), ALU.mult, ["oh", ("ps", 5)], ["pr"])
                rk = rt[:, 100:104]
                P.op("dve", lambda e, rk=rk, pr=pr: e.reduce_sum(rk, pr, AX.X), ["pr"], ["rk"])
                posf = rt[:, 104:108]
                STT("dve", posf, idxf, float(CAP), rk, ALU.mult, ALU.add, ["idxf", "rk"], ["posf"])
                CP("dve", posi[:, c, :], posf, ["posf"], ["posi"])
                pw = rt[:, 384:512].rearrange("p (k e) -> p k e", k=4)
                TT("dve", pw, oh, wts[:, c, :].unsqueeze(2).to_broadcast([128, 4, 32]), ALU.mult, ["oh", "wts"], ["pw"])
                wdn = rt[:, 512:544]
                P.op("dve", lambda e, wdn=wdn, pw=pw: e.reduce_sum(wdn, pw.rearrange("p k e -> p e k"), AX.X), ["pw"], ["wdn"])
                TR(PS[6][0:32, 0:128], wdn, ident[:], ["wdn"], [("ps", 6)])
                CP("act", WdT[:, c * 128:(c + 1) * 128], PS[6][0:32, 0:128], [("ps", 6)], ["WdT"])
                for k4 in range(4):
                    P.dma("pool", lambda e, c=c, k4=k4, h1t=h1t: e.indirect_dma_start(
                        out=XS_d, out_offset=bass.IndirectOffsetOnAxis(ap=posi[:, c, k4:k4 + 1], axis=0),
                        in_=h1t, in_offset=None), [htok, "posi"], [("XS", c, k4)])

            P.barrier()
            ring5 = [UA[:, i * 4096:(i + 1) * 4096].rearrange("p (k n) -> p k n", k=8) for i in range(5)]
            rs5 = {"n": 0}

            def load5(src_ap):
                i = rs5["n"] % 5
                rs5["n"] += 1
                DMA("sp", ring5[i], src_ap, [], [("r5", i)])
                return ring5[i], ("r5", i)

            def eunit(w_ap, e, hf):
                return w_ap[e, :, hf * 512:(hf + 1) * 512].rearrange("(k p) n -> p k n", p=128)

            gTb = [gT, rcp[:, 0:CAP]]
            sgTb = [sgT, bcs[:, 0:CAP]]
            uTb = [uT, rt[:, 0:CAP]]
            t1Tb = [t1T, UC[:, 6656:6656 + CAP]]
            for e_ in range(NE):
                if e_ == 0:
                    DMA("act", Xe, XS_d[0:CAP, :].rearrange("(j p) d -> p j d", p=128), [], ["Xe"])
                ug = [None, None]
                uu_ = [None, None]
                ug[0] = load5(eunit(wg_d, e_, 0))
                uu_[0] = load5(eunit(wu_d, e_, 0))
                for k in range(8):
                    pst = PS[6 + (k % 2)]
                    for j in range(3):
                        TR(pst[:, j * 128:(j + 1) * 128], Xe[:, j, k * 128:(k + 1) * 128], ident[:], ["Xe"], [("ps", 6 + (k % 2))])
                    CP("act" if k % 2 == 0 else "dve", xTe[:, k, :], pst[:, 0:384], [("ps", 6 + (k % 2))], ["xTe"])
                if e_ + 1 < NE:
                    DMA("act", Xe, XS_d[(e_ + 1) * CAP:(e_ + 2) * CAP, :].rearrange("(j p) d -> p j d", p=128), [], ["Xe"])
                ug[1] = load5(eunit(wg_d, e_, 1))
                uu_[1] = load5(eunit(wu_d, e_, 1))
                for m in range(8):
                    hf, mm_ = m // 4, m % 4
                    (gu, gt), (uu, utk) = ug[hf], uu_[hf]
                    psg = PS[(m % 2) * 2]
                    psu = PS[(m % 2) * 2 + 1]
                    for k in range(8):
                        MM(psg[:, 0:CAP], gu[:, k, mm_ * 128:(mm_ + 1) * 128], xTe[:, k, :], k == 0, k == 7, [gt, "xTe"], [("ps", (m % 2) * 2)])
                    for k in range(8):
                        MM(psu[:, 0:CAP], uu[:, k, mm_ * 128:(mm_ + 1) * 128], xTe[:, k, :], k == 0, k == 7, [utk, "xTe"], [("ps", (m % 2) * 2 + 1)])
                    pb_ = m % 2
                    gT_, sgT_, uT_, t1T_ = gTb[pb_], sgTb[pb_], uTb[pb_], t1Tb[pb_]
                    TS("dve", gT_, psg[:, 0:CAP], bgT[:, m, e_:e_ + 1], 7.0, ALU.add, ALU.min, [("ps", (m % 2) * 2), "bgT"], [("gT", pb_)])
                    ACTF(sgT_, gT_, AF.Sigmoid, [("gT", pb_)], [("sgT", pb_)], scale=1.702)
                    TS("dve", uT_, psu[:, 0:CAP], buT[:, m, e_:e_ + 1], 7.0, ALU.add, ALU.min, [("ps", (m % 2) * 2 + 1), "buT"], [("uT", pb_)])
                    TS("dve", uT_, uT_, -7.0, 1.0, ALU.max, ALU.add, [("uT", pb_)], [("uT", pb_)])
                    TT("dve", t1T_, gT_, sgT_, ALU.mult, [("gT", pb_), ("sgT", pb_)], [("t1T", pb_)])
                    TT("dve", actT[:, m, :], t1T_, uT_, ALU.mult, [("t1T", pb_), ("uT", pb_)], ["actT"])
                ud = [load5(eunit(wd_d, e_, 0)), load5(eunit(wd_d, e_, 1))]
                for t in range(3):
                    yt = ytile[t % 2]
                    ytok = ("yt", t % 2)
                    for hf in range(2):
                        u, ut = ud[hf]
                        psy = PS[4 + hf]
                        for k in range(8):
                            MM(psy[:], actT[:, k, t * 128:(t + 1) * 128], u[:, k, :], k == 0, k == 7, [ut, "actT"], [("ps", 4 + hf)])
                        CP("act", yt[:, hf * 512:(hf + 1) * 512], psy[:], [("ps", 4 + hf)], [ytok])
                    DMA("act", YS_d[e_ * CAP + t * 128:e_ * CAP + (t + 1) * 128, :], yt, [ytok], [("YS", e_, t)])

            P.barrier()
            DMA("sp", lnG[:], lnp_d[4:5, :].to_broadcast([128, D]), [], ["lnG"])
            DMA("sp", lnB[:], lnp_d[5:6, :].to_broadcast([128, D]), [], ["lnB"])
            gsets = [
                [UC[:, 0:1024], UC[:, 1024:2048], UC[:, 2048:3072], UC[:, 3072:4096]],
                [UC[:, 4096:5120], UC[:, 5120:6144], UC[:, 6144:7168], wk[:, 2, :]],
            ]
            for c in range(NCH):
                par = c % 2
                h1t = wk[:, par, :]
                htok = ("wk", par)
                DMA("sp", h1t, H1_d[c * 128:(c + 1) * 128, :], [], [htok])
                gts = []
                for k4 in range(4):
                    gt_ = gsets[par][k4]
                    gk_ = ("ga", par, k4)
                    P.dma("pool", lambda e, c=c, k4=k4, gt_=gt_: e.indirect_dma_start(
                        out=gt_, out_offset=None, in_=YS_d,
                        in_offset=bass.IndirectOffsetOnAxis(ap=posi[:, c, k4:k4 + 1], axis=0)), ["posi"], [gk_])
                    gts.append((gt_, gk_))
                for hf in range(2):
                    MM(PS[2 * par + hf][:], WdT[:, c * 128:(c + 1) * 128], bdn[:, hf * 512:(hf + 1) * 512], True, True,
                       ["WdT", "bdn"], [("ps", 2 * par + hf)])
                acc, atok = gts[0]
                TS("dve", acc, acc, wts[:, c, 0:1], None, ALU.mult, None, [atok, "wts"], [atok])
                for k4 in range(1, 4):
                    STT("dve", acc, gts[k4][0], wts[:, c, k4:k4 + 1], acc, ALU.mult, ALU.add,
                        [gts[k4][1], "wts", atok], [atok])
                for hf in range(2):
                    TT("dve", acc[:, hf * 512:(hf + 1) * 512], acc[:, hf * 512:(hf + 1) * 512], PS[2 * par + hf][:], ALU.add,
                       [atok, ("ps", 2 * par + hf)], [atok])
                STT("dve", acc, h1t, ALPHA, acc, ALU.mult, ALU.add, [htok, atok], [atok])
                layer_norm(acc, acc, acc, [atok], [atok], "ln2")
                outs.append(DMA("act", out_d[c * 128:(c + 1) * 128, :], acc, [atok], [("out", c)]))
        if stage < 3:
            fw = {"act": list(P.dma_ops["act"][-10:]), "sp": list(P.dma_ops["sp"][-10:]), "pool": list(P.dma_ops["pool"][-10:])}
        else:
            fw = {"act": outs}
        P.emit(fw)
    return nc


def _prep_inputs(inp):
    f = lambda a: np.ascontiguousarray(np.asarray(a, dtype=np.float32))
    w_in = f(inp["w_in"])[0]
    qa, ka, va, qg, kg, vg, ga, gb = np.split(w_in, np.cumsum([512, 128, 128, 512, 128, 128, 1024])[:], axis=1)

    def qperm(q):
        cols = []
        for j in range(4):
            cols.append(q[:, j * 64:(j + 1) * 64])
            cols.append(q[:, (j + 4) * 64:(j + 5) * 64])
        return np.concatenate(cols, axis=1)

    w_inp = np.ascontiguousarray(np.concatenate([qperm(qa), qperm(qg), ka, kg, va, vg, ga, gb], axis=1))

    def bperm(w):
        rows = []
        for j in range(4):
            rows.append(w[j * 64:(j + 1) * 64])
            rows.append(w[(j + 4) * 64:(j + 5) * 64])
        return np.ascontiguousarray(np.concatenate(rows, axis=0))

    lnp = np.stack([f(inp["ln0_g"]), f(inp["ln0_b"]), f(inp["ln1_g"])[0], f(inp["ln1_b"])[0],
                    f(inp["ln2_g"])[0], f(inp["ln2_b"])[0]], axis=0)
    small = np.concatenate([f(inp["a_sink"])[0], f(inp["b_q_norm"])[0], f(inp["b_k_norm"])[0],
                            f(inp["b_router"])[0]])[None, :]
    shared = {
        "lnp": np.ascontiguousarray(lnp), "w_in": w_inp, "small": np.ascontiguousarray(small),
        "w_ba": bperm(f(inp["w_branch_a"])[0]), "w_bb": bperm(f(inp["w_branch_b"])[0]),
        "w_out": f(inp["w_out"])[0], "w_router": f(inp["w_router"])[0],
        "w_gate": f(inp["w_gate"])[0], "w_up": f(inp["w_up"])[0], "w_down": f(inp["w_down"])[0],
        "b_gate": f(inp["b_gate"])[0], "b_up": f(inp["b_up"])[0], "b_down": f(inp["b_down"])[0],
    }
    shared.update(_consts())
    return shared


def kernel(**inputs):
    shared = _prep_inputs(inputs)
    x = np.asarray(inputs["x"], dtype=np.float32)
    n = x.shape[0]
    nc = build_nc(3)
    in_maps = []
    for b in range(n):
        m = dict(shared)
        m["x"] = np.ascontiguousarray(x[b])
        in_maps.append(m)
    res = run_bass_kernel_spmd(nc, in_maps, core_ids=list(range(n)))
    return np.stack([np.asarray(r["out"], dtype=np.float32) for r in res.results], axis=0)
```

```python
import contextlib
import os
import numpy as np
import concourse.bass as bass
import concourse.mybir as mybir
from concourse.bass_utils import run_bass_kernel_spmd

F32 = mybir.dt.float32
F32R = mybir.dt.float32r
U32 = mybir.dt.uint32
I32 = mybir.dt.int32
ALU = mybir.AluOpType
AF = mybir.ActivationFunctionType
AX = mybir.AxisListType

S = 2048
D = 1024
NCH = 16
NG = 4
NE = 32
CAP = 384
NSLOT = NE * CAP
ALPHA = 2.0 ** 0.25
LN_EPS = 1e-5
RMS_EPS = 1e-6
MASKV = -30000.0
ENGS = ["pe", "act", "dve", "pool", "sp"]


class Op:
    __slots__ = ("eng", "fn", "deps", "signal", "sig_idx", "is_dma", "dma_sem", "dma_val")

    def __init__(self, eng, fn, is_dma):
        self.eng = eng
        self.fn = fn
        self.deps = []
        self.signal = False
        self.sig_idx = None
        self.is_dma = is_dma
        self.dma_sem = None
        self.dma_val = None


class Prog:
    def __init__(self, nc, dma_ring=10):
        self.nc = nc
        self.ops = {e: [] for e in ENGS}
        self.tok = {}
        self.dma_ring = dma_ring
        self.dma_count = {e: 0 for e in ENGS}
        self.dma_ops = {e: [] for e in ENGS}

    def _track(self, o, reads, writes):
        for t in reads:
            st = self.tok.get(t)
            if st is None:
                st = self.tok[t] = [None, []]
            if st[0] is not None:
                o.deps.append((st[0], "raw"))
            st[1].append(o)
        for t in writes:
            st = self.tok.get(t)
            if st is None:
                st = self.tok[t] = [None, []]
            if st[0] is not None:
                o.deps.append((st[0], "waw"))
            for r in st[1]:
                if r is not o:
                    o.deps.append((r, "war"))
            st[0] = o
            st[1] = []

    def op(self, eng, fn, reads=(), writes=()):
        o = Op(eng, fn, False)
        self._track(o, reads, writes)
        self.ops[eng].append(o)
        return o

    def dma(self, eng, fn, reads=(), writes=()):
        o = Op(eng, fn, True)
        self._track(o, reads, writes)
        i = self.dma_count[eng]
        self.dma_count[eng] += 1
        o.dma_sem = (eng, i % self.dma_ring)
        o.dma_val = 16 * (i // self.dma_ring + 1)
        if i >= self.dma_ring:
            o.deps.append((self.dma_ops[eng][i - self.dma_ring], "ring"))
        self.dma_ops[eng].append(o)
        self.ops[eng].append(o)
        return o

    def barrier(self):
        lasts = []
        for e in ENGS:
            comp = [o for o in self.ops[e] if not o.is_dma and o.fn is not None]
            if comp:
                lasts.append(comp[-1])
            lasts.extend(self.dma_ops[e][-self.dma_ring:])
        for e in ENGS:
            o = Op(e, None, False)
            for l in lasts:
                o.deps.append((l, "bar"))
            self.ops[e].append(o)
        self.tok = {}

    @staticmethod
    def _skip(o, d, kind):
        if d.is_dma:
            return False
        if d.eng == o.eng and not o.is_dma and kind != "bar":
            if d.eng == "pe" or kind != "raw":
                return True
        return False

    def emit(self, final_wait):
        nc = self.nc
        for e in ENGS:
            for o in self.ops[e]:
                for d, kind in o.deps:
                    if d.is_dma or self._skip(o, d, kind):
                        continue
                    d.signal = True
        for e in ENGS:
            c = 0
            for o in self.ops[e]:
                if o.signal:
                    c += 1
                    o.sig_idx = c
        with contextlib.ExitStack() as st:
            st.enter_context(nc.cleanup_on_exit())
            esem = {e: nc.alloc_semaphore(name="s_" + e) for e in ENGS}
            dsem = {}
            for e in ENGS:
                for i in range(min(self.dma_ring, self.dma_count[e])):
                    dsem[(e, i)] = nc.alloc_semaphore(name="d_%s_%d" % (e, i))
            for sm in list(esem.values()) + list(dsem.values()):
                nc.gpsimd.sem_clear(sm)
            block = st.enter_context(nc.Block())

            def run(e, eng):
                seen = {}
                for o in self.ops[e]:
                    need = {}
                    for d, kind in o.deps:
                        if self._skip(o, d, kind):
                            continue
                        if d.is_dma:
                            key, sem, val = ("d",) + d.dma_sem, dsem[d.dma_sem], d.dma_val
                        else:
                            key, sem, val = ("e", d.eng), esem[d.eng], d.sig_idx
                        if seen.get(key, 0) >= val:
                            continue
                        if key not in need or need[key][1] < val:
                            need[key] = (sem, val)
                    for key, (sem, val) in need.items():
                        eng.wait_ge(sem, val)
                        seen[key] = val
                    if o.fn is None:
                        continue
                    ins = o.fn(eng)
                    if o.is_dma:
                        ins.then_inc(dsem[o.dma_sem], 16)
                    elif o.signal:
                        ins.then_inc(esem[e], 1)
                for d in final_wait.get(e, []):
                    eng.wait_ge(dsem[d.dma_sem], d.dma_val)

            block.tensor(lambda eng: run("pe", eng))
            block.scalar(lambda eng: run("act", eng))
            block.vector(lambda eng: run("dve", eng))
            block.gpsimd(lambda eng: run("pool", eng))
            block.sync(lambda eng: run("sp", eng))


def _consts():
    c = {}
    c["ident"] = np.eye(128, dtype=np.float32)
    bo = np.zeros((128, 128), np.float32)
    bo[:64, :64] = 1.0 / 64
    bo[64:, 64:] = 1.0 / 64
    c["blockones"] = bo
    rot = np.zeros((128, 128), np.float32)
    for m in range(128):
        r = (m % 64) % 32
        if r < 16:
            rot[m + 16, m] = -1.0
        else:
            rot[m - 16, m] = 1.0
    c["rot"] = rot
    t = np.arange(S)
    row = (t // 64).astype(np.float32)
    col = (t % 64).astype(np.float32)
    inv = (np.float32(10000.0) ** (-np.arange(16, dtype=np.float32) * np.float32(2.0 / 32))).astype(np.float32)
    cos = np.zeros((128, S), np.float32)
    sin = np.zeros((128, S), np.float32)
    for p in range(128):
        f = p % 64
        pos = row if f < 32 else col
        ang = (pos * inv[f % 16]).astype(np.float32)
        cos[p] = np.cos(ang)
        sin[p] = np.sin(ang)
    c["rcos"] = cos
    c["rsin"] = sin
    slopes = np.array([2.0 ** (-8.0 * (h + 1) / 8) for h in range(8)], np.float32)
    bias = np.zeros((128, 3, 2, 4, 128), np.float32)
    ki = np.arange(128)[:, None]
    qi = np.arange(128)[None, :]
    for oi, o in enumerate((-1, 0, 1)):
        dist = np.abs(qi - ki - 128 * o)
        for kv in range(2):
            for j in range(4):
                h = j + 4 * kv
                bias[:, oi, kv, j, :] = np.where(dist <= 128, -slopes[h] * dist.astype(np.float32), MASKV)
    c["abias"] = bias.reshape(128, 6 * 512)
    tri = (np.arange(128)[:, None] < np.arange(128)[None, :]).astype(np.float32)
    c["tri"] = tri
    c["iota32"] = np.tile(np.arange(32, dtype=np.float32)[None, :], (128, 1))
    return c


def build_nc(stage=3, stop=None):
    nc = bass.Bass("TRN2", target_bir_lowering=False)
    nc.dge_precook = False

    def din(name, shape, dt=F32):
        return nc.dram_tensor(name, list(shape), dt, kind="ExternalInput").ap()

    x_d = din("x", [S, D])
    lnp_d = din("lnp", [6, D])
    win_d = din("w_in", [D, 3584], F32R)
    small_d = din("small", [1, 8 + 64 + 64 + 32])
    wba_d = din("w_ba", [512, D], F32R)
    wbb_d = din("w_bb", [512, D], F32R)
    wout_d = din("w_out", [D, D], F32R)
    wr_d = din("w_router", [D, NE])
    wg_d = din("w_gate", [NE, D, D], F32R)
    wu_d = din("w_up", [NE, D, D], F32R)
    wd_d = din("w_down", [NE, D, D], F32R)
    bg_d = din("b_gate", [NE, D])
    bu_d = din("b_up", [NE, D])
    bd_d = din("b_down", [NE, D])
    c_ident = din("ident", [128, 128])
    c_bo = din("blockones", [128, 128], F32R)
    c_rot = din("rot", [128, 128], F32R)
    c_cos = din("rcos", [128, S])
    c_sin = din("rsin", [128, S])
    c_abias = din("abias", [128, 3072])
    c_tri = din("tri", [128, 128])
    c_iota = din("iota32", [128, 32])
    out_d = nc.dram_tensor("out", [S, D], F32, kind="ExternalOutput").ap()
    dbg_d = None
    if stage < 3:
        dbg_d = nc.dram_tensor("dbg", [S, D], F32, kind="ExternalOutput").ap()
    H0_d = nc.dram_tensor("H0", [S, D], F32, kind="Internal").ap()
    H0T_d = nc.dram_tensor("H0T", [NG, 128, 8 * 512], F32R, kind="Internal").ap()
    H1_d = nc.dram_tensor("H1", [S, D], F32, kind="Internal").ap()
    XS_d = nc.dram_tensor("XS", [NSLOT, D], F32, kind="Internal").ap()
    YS_d = nc.dram_tensor("YS", [NSLOT, D], F32, kind="Internal").ap()

    with contextlib.ExitStack() as st:
        def sb(name, shape, dt=F32):
            return st.enter_context(nc.sbuf_tensor(name, list(shape), dt))

        def psb(name):
            return st.enter_context(nc.psum_tensor(name, [128, 512], F32))

        UA = sb("UA", [128, 20480], F32R)
        UB = sb("UB", [128, 8320], F32R)
        expb = sb("expb", [128, 3, 512], F32R)
        tmpR = sb("tmpR", [128, 2, 512], F32R)
        tmpO = sb("tmpO", [128, 512], F32R)
        expb2 = sb("expb2", [128, 512], F32R)
        cbo = sb("cbo", [128, 128], F32R)
        crot = sb("crot", [128, 128], F32R)
        UC = sb("UC", [128, 7168], F32)
        wk = sb("wk", [128, 4, 1024], F32)
        lnG = sb("lnG", [128, 1024], F32)
        lnB = sb("lnB", [128, 1024], F32)
        ident = sb("identt", [128, 128], F32)
        tri = sb("trit", [128, 128], F32)
        onesM = sb("onesM", [128, 128], F32)
        iota = sb("iotat", [128, 32], F32)
        smallb = sb("smallb", [128, 168], F32)
        esink = sb("esinkt", [128, 8], F32)
        gqk = sb("gqk", [128, 2], F32)
        stats = sb("stats", [128, 2, 6], F32)
        mv = sb("mvt", [128, 2], F32)
        lnt = sb("lnt", [128, 4], F32)
        rcp = sb("rcp", [128, 512], F32)
        bcs = sb("bcs", [128, 512], F32)
        WdT = sb("WdT", [32, 2048], F32)
        bdn = sb("bdn", [32, 1024], F32)
        bgT = sb("bgT", [128, 8, 32], F32)
        buT = sb("buT", [128, 8, 32], F32)
        wrt = sb("wrt", [128, 8, 32], F32)
        rt = sb("rt", [128, 640], F32)
        rti = sb("rti", [128, 8], U32)
        posi = sb("posi", [128, NCH, 4], I32)
        wts = sb("wts", [128, NCH, 4], F32)
        msum = sb("msum", [128, 32], F32)
        PS = [psb("ps%d" % i) for i in range(8)]

        P = Prog(nc)
        print("sbuf bytes remaining", nc.sbuf_bytes_remaining)

        def MM(out, lhsT, rhs, start, stop, r, w):
            P.op("pe", lambda e: e.matmul(out, lhsT, rhs, start=start, stop=stop), r, w)

        def TR(out, in_, idn, r, w):
            P.op("pe", lambda e: e.transpose(out, in_, idn), list(r) + ["ident"], w)

        def ACTF(out, in_, func, r, w, bias=0.0, scale=1.0, accum=None):
            if accum is None:
                P.op("act", lambda e: e.activation(out, in_, func, bias=bias, scale=scale), r, w)
            else:
                P.op("act", lambda e: e.activation(out, in_, func, bias=bias, scale=scale, accum_out=accum), r, w)

        def TT(eng, out, a, b, op, r, w):
            P.op(eng, lambda e: e.tensor_tensor(out, a, b, op), r, w)

        def TS(eng, out, a, s1, s2, op0, op1, r, w):
            if s2 is None:
                P.op(eng, lambda e: e.tensor_scalar(out, a, s1, None, op0), r, w)
            else:
                P.op(eng, lambda e: e.tensor_scalar(out, a, s1, s2, op0, op1), r, w)

        def STT(eng, out, in0, scalar, in1, op0, op1, r, w):
            P.op(eng, lambda e: e.scalar_tensor_tensor(out, in0, scalar, in1, op0, op1), r, w)

        def CP(eng, out, in_, r, w):
            if eng == "act":
                P.op("act", lambda e: e.copy(out, in_), r, w)
            else:
                P.op(eng, lambda e: e.tensor_copy(out, in_), r, w)

        def DMA(q, out, in_, r, w):
            return P.dma(q, lambda e: e.dma_start(out=out, in_=in_), r, w)

        def layer_norm(src, dst, tmp, r, w, tag):
            P.op("dve", lambda e: e.bn_stats(stats[:, 0, :], src[:, 0:512]), r, ["st0"])
            P.op("dve", lambda e: e.bn_stats(stats[:, 1, :], src[:, 512:1024]), r, ["st1"])
            P.op("dve", lambda e: e.bn_aggr(mv[:], stats[:]), ["st0", "st1"], ["mv"])
            ACTF(lnt[:, 0:1], mv[:, 1:2], AF.Sqrt, ["mv"], ["ln_sd"], bias=LN_EPS)
            P.op("dve", lambda e: e.reciprocal(lnt[:, 1:2], lnt[:, 0:1]), ["ln_sd"], ["ln_rstd"])
            STT("dve", tmp, src, mv[:, 0:1], lnG[:], ALU.subtract, ALU.mult, list(r) + ["mv", "lnG"], [tag + "_t"])
            STT("dve", dst, tmp, lnt[:, 1:2], lnB[:], ALU.mult, ALU.add, [tag + "_t", "ln_rstd", "lnB"], w)

        ring = [UA[:, i * 4096:(i + 1) * 4096].rearrange("p (k n) -> p k n", k=8) for i in range(5)]
        h0Tg = UA[:, 8192:12288].rearrange("p (k n) -> p k n", k=8)
        qAT = UA[:, 12288:14336].rearrange("p (j n) -> p j n", j=4)
        qBT = UA[:, 14336:16384].rearrange("p (j n) -> p j n", j=4)
        OAT = UA[:, 16384:18432].rearrange("p (j n) -> p j n", j=4)
        OBT = UA[:, 18432:20480].rearrange("p (j n) -> p j n", j=4)
        mrg = UA[:, 12288:16384].rearrange("p (k n) -> p k n", k=8)
        kAT = UB[:, 0:2048]
        kBT = UB[:, 2048:4096]
        vAa = UB[:, 4096:6208].rearrange("p (c g d) -> p c g d", c=16, g=2)
        vBa = UB[:, 6208:8320].rearrange("p (c g d) -> p c g d", c=16, g=2)
        xTe = UB[:, 0:3072].rearrange("p (k n) -> p k n", k=8)
        actT = UB[:, 3072:6144].rearrange("p (k n) -> p k n", k=8)
        rcosg = UC[:, 0:512]
        rsing = UC[:, 512:1024]
        abias = UC[:, 1024:4096].rearrange("p (o n) -> p o n", o=6)
        t_rstd = UC[:, 4096:4608]
        t_a = UC[:, 4608:5120]
        t_b = UC[:, 5120:5632]
        t_sig = UC[:, 5632:6144]
        t_sp0 = UC[:, 6144:6656]
        t_sp1 = UC[:, 6656:7168]
        Xe = UC[:, 0:3072].rearrange("p (j d) -> p j d", j=3)
        gT = UC[:, 3072:3456]
        sgT = UC[:, 3456:3840]
        uT = UC[:, 3840:4224]
        t1T = UC[:, 4224:4608]
        ytile = [UC[:, 4608:5632], UC[:, 5632:6656]]

        DMA("sp", ident[:], c_ident, [], ["ident"])
        DMA("sp", cbo[:], c_bo, [], ["cbo"])
        DMA("sp", crot[:], c_rot, [], ["crot"])
        DMA("sp", tri[:], c_tri, [], ["tri"])
        DMA("sp", iota[:], c_iota, [], ["iota"])
        DMA("sp", smallb[:], small_d.to_broadcast([128, 168]), [], ["smallb"])
        DMA("sp", abias, c_abias.rearrange("p (o n) -> p o n", o=6), [], ["abias"])
        DMA("sp", lnG[:], lnp_d[0:1, :].to_broadcast([128, D]), [], ["lnG"])
        DMA("sp", lnB[:], lnp_d[1:2, :].to_broadcast([128, D]), [], ["lnB"])
        P.op("pool", lambda e: e.memset(onesM[:], 1.0), [], ["onesM"])
        ACTF(esink[:], smallb[:, 0:8], AF.Exp, ["smallb"], ["esink"])
        gsel = rt[:, 0:64]
        TT("dve", gsel, smallb[:, 8:72], ident[:, 0:64], ALU.mult, ["smallb", "ident"], ["gsel"])
        TT("dve", rt[:, 64:128], smallb[:, 8:72], ident[:, 64:128], ALU.mult, ["smallb", "ident"], ["gsel2"])
        TT("dve", gsel, gsel, rt[:, 64:128], ALU.add, ["gsel", "gsel2"], ["gsel3"])
        P.op("dve", lambda e: e.reduce_sum(gqk[:, 0:1], gsel, AX.X), ["gsel3"], ["gq"])
        gsel_k = rt[:, 128:192]
        TT("dve", gsel_k, smallb[:, 72:136], ident[:, 0:64], ALU.mult, ["smallb", "ident"], ["gselk"])
        TT("dve", rt[:, 192:256], smallb[:, 72:136], ident[:, 64:128], ALU.mult, ["smallb", "ident"], ["gselk2"])
        TT("dve", gsel_k, gsel_k, rt[:, 192:256], ALU.add, ["gselk", "gselk2"], ["gselk3"])
        P.op("dve", lambda e: e.reduce_sum(gqk[:, 1:2], gsel_k, AX.X), ["gselk3"], ["gk"])
        ones_v = onesM[:, 0:32].rearrange("p (c g d) -> p c g d", c=16, g=2)
        CP("dve", vAa[:, :, :, 64:65], ones_v, ["onesM"], ["vA_ones"])
        CP("dve", vBa[:, :, :, 64:65], ones_v, ["onesM"], ["vB_ones"])

        def rms_rope(ps_in, ps_tok, gcol, dst, dst_tok, bset=0):
            if bset == 0:
                sq, sqk, kn, knk = tmpR[:, 0, :], "tmpR0", tmpR[:, 1, :], "tmpR1"
                rs, rsk, ta, tak, tb, tbk = t_rstd, "t_rstd", t_a, "t_a", t_b, "t_b"
                p6, p7 = 6, 7
            else:
                sq, sqk, kn, knk = expb[:, 0, :], ("expb", 0), expb[:, 1, :], ("expb", 1)
                rs, rsk, ta, tak, tb, tbk = t_sig, "t_sig", t_sp0, "t_sp0", t_sp1, "t_sp1"
                p6, p7 = 2, 3
            ACTF(sq, ps_in, AF.Square, [ps_tok], [sqk])
            MM(PS[p6][:], cbo[:], sq, True, True, ["cbo", sqk], [("ps", p6)])
            ACTF(rs, PS[p6][:], AF.Sqrt, [("ps", p6)], [rsk], bias=RMS_EPS)
            P.op("dve", lambda e: e.reciprocal(rs, rs), [rsk], [rsk])
            STT("dve", kn, ps_in, gcol, rs, ALU.mult, ALU.mult, [ps_tok, rsk, "gq", "gk"], [knk])
            MM(PS[p7][:], crot[:], kn, True, True, ["crot", knk], [("ps", p7)])
            TT("pool", ta, kn.bitcast(F32), rcosg, ALU.mult, [knk, "rope"], [tak])
            TT("dve", tb, PS[p7][:], rsing, ALU.mult, [("ps", p7), "rope"], [tbk])
            TT("dve", dst, ta, tb, ALU.add, [tak, tbk], dst_tok)

        kv_unit = ring[0]
        p1_groups = NG if stop not in ("c0", "ln", "tr", "k", "kr", "v") else (0 if stop == "c0" else 1)
        DMA("sp", kv_unit, win_d[:, 1024:1536].rearrange("(k p) n -> p k n", p=128), [], [("ring", 0)])
        for g in range(p1_groups):
            DMA("sp", rcosg, c_cos[:, g * 512:(g + 1) * 512], [], ["rope"])
            DMA("sp", rsing, c_sin[:, g * 512:(g + 1) * 512], [], ["rope"])
            for j in range(4):
                c = g * 4 + j
                xt = wk[:, j, :]
                DMA("sp", xt, x_d[c * 128:(c + 1) * 128, :], [], [("wk", j)])
                layer_norm(xt, xt, xt, [("wk", j)], [("wk", j)], "ln0")
                DMA("act", H0_d[c * 128:(c + 1) * 128, :], xt, [("wk", j)], [("H0", c)])
                if stop == "ln":
                    continue
                for half in range(2):
                    pst = PS[2 + half]
                    for kk in range(4):
                        k = half * 4 + kk
                        TR(pst[:, kk * 128:(kk + 1) * 128], xt[:, k * 128:(k + 1) * 128], ident[:], [("wk", j)], [("ps", 2 + half)])
                    CP("act" if half == 0 else "dve",
                       h0Tg[:, half * 4:(half + 1) * 4, j * 128:(j + 1) * 128],
                       pst[:].rearrange("p (k n) -> p k n", k=4), [("ps", 2 + half)], ["h0Tg"])
            if stop == "ln":
                continue
            DMA("act", H0T_d[g], UA[:, 8192:12288], ["h0Tg"], [("H0T", g)])
            if stop == "tr":
                continue
            for blk in range(2):
                psk = PS[blk]
                for k in range(8):
                    MM(psk[:], kv_unit[:, k, blk * 128:(blk + 1) * 128], h0Tg[:, k, :], k == 0, k == 7,
                       [("ring", 0), "h0Tg"], [("ps", blk)])
            CP("act", kAT[:, g * 512:(g + 1) * 512], PS[0][:], [("ps", 0)], ["kAT"])
            if stop == "k":
                continue
            rms_rope(PS[1][:], ("ps", 1), gqk[:, 1:2], kBT[:, g * 512:(g + 1) * 512], ["kBT"])
            if stop == "kr":
                continue
            for j in range(4):
                c = g * 4 + j
                psv = PS[4 + (j % 2)]
                for k in range(8):
                    MM(psv[:, 0:256], h0Tg[:, k, j * 128:(j + 1) * 128], kv_unit[:, k, 256:512], k == 0, k == 7,
                       [("ring", 0), "h0Tg"], [("ps", 4 + (j % 2))])
                veng = "act" if j % 2 == 0 else "dve"
                CP(veng, vAa[:, c, :, 0:64], psv[:, 0:128].rearrange("p (g d) -> p g d", g=2),
                   [("ps", 4 + (j % 2))], ["vA"])
                CP(veng, vBa[:, c, :, 0:64], psv[:, 128:256].rearrange("p (g d) -> p g d", g=2),
                   [("ps", 4 + (j % 2))], ["vB"])

        DMA("sp", lnG[:], lnp_d[2:3, :].to_broadcast([128, D]), [], ["lnG"])
        DMA("sp", lnB[:], lnp_d[3:4, :].to_broadcast([128, D]), [], ["lnB"])
        rstate = {"n": 0}

        def load_unit(src_ap, kch=8):
            i = rstate["n"] % 2
            rstate["n"] += 1
            DMA("sp", ring[i][:, 0:kch, :], src_ap, [], [("ring", i)])
            return ring[i], ("ring", i)

        def wunit(w_ap, c0, kchunks=8):
            return w_ap[:, c0:c0 + 512].rearrange("(k p) n -> p k n", p=128)

        def phase2():
            for g in range(NG):
                DMA("sp", UA[:, 8192:12288], H0T_d[g], [("H0T", g)], ["h0Tg"])
                DMA("sp", rcosg, c_cos[:, g * 512:(g + 1) * 512], [], ["rope"])
                DMA("sp", rsing, c_sin[:, g * 512:(g + 1) * 512], [], ["rope"])
                u, ut = load_unit(wunit(win_d, 0))
                for j in range(4):
                    psq = PS[j % 2]
                    for k in range(8):
                        MM(psq[:], u[:, k, j * 128:(j + 1) * 128], h0Tg[:, k, :], k == 0, k == 7, [ut, "h0Tg"], [("ps", j % 2)])
                    CP("act", qAT[:, j, :], psq[:], [("ps", j % 2)], ["qAT", ("mrg", j)])
                u, ut = load_unit(wunit(win_d, 512))
                for j in range(4):
                    psq = PS[j % 2]
                    for k in range(8):
                        MM(psq[:], u[:, k, j * 128:(j + 1) * 128], h0Tg[:, k, :], k == 0, k == 7, [ut, "h0Tg"], [("ps", j % 2)])
                    rms_rope(psq[:], ("ps", j % 2), gqk[:, 0:1], qBT[:, j, :], ["qBT", ("mrg", 4 + j)], bset=j % 2)

                if stop == 'q':
                    return
                bufsets = [
                    dict(banks=[2, 3, 4], ex=[expb[:, 0, :], expb[:, 1, :], expb[:, 2, :]],
                         extok=[("expb", 0), ("expb", 1), ("expb", 2)],
                         ts=[t_a, t_b, t_sig], tstok=["t_a", "t_b", "t_sig"]),
                    dict(banks=[0, 1, 6], ex=[tmpR[:, 0, :], tmpR[:, 1, :], expb2[:]],
                         extok=["tmpR0", "tmpR1", ("expb", 5)],
                         ts=[t_rstd, t_sp0, t_sp1], tstok=["t_rstd", "t_sp0", "t_sp1"]),
                ]

                def a_stage_x(nq, kv, bs):
                    n = g * 4 + nq
                    offs = [o for o in (-1, 0, 1) if 0 <= n + o < NCH]
                    pb = slice(kv * 64, (kv + 1) * 64)
                    for oi, o in enumerate(offs):
                        c = n + o
                        bk = bs["banks"][oi]
                        pss = PS[bk]
                        for j in range(4):
                            MM(pss[:, j * 128:(j + 1) * 128], kAT[pb, c * 128:(c + 1) * 128],
                               qAT[pb, j, nq * 128:(nq + 1) * 128], True, True, ["kAT", "qAT"], [("ps", bk)])
                        STT("dve", bs["ts"][oi], pss[:], 0.125, abias[:, (o + 1) * 2 + kv, :], ALU.mult, ALU.add,
                            [("ps", bk), "abias"], [bs["tstok"][oi]])
                        ACTF(bs["ex"][oi], bs["ts"][oi], AF.Exp, [bs["tstok"][oi]], [bs["extok"][oi]])

                def a_stage_y(nq, kv, bs):
                    n = g * 4 + nq
                    offs = [o for o in (-1, 0, 1) if 0 <= n + o < NCH]
                    pso = PS[5]
                    for j in range(4):
                        for oi, o in enumerate(offs):
                            c = n + o
                            MM(pso[0:65, j * 128:(j + 1) * 128], vAa[:, c, kv, 0:65], bs["ex"][oi][:, j * 128:(j + 1) * 128],
                               oi == 0, oi == len(offs) - 1, ["vA", "vA_ones", bs["extok"][oi]], [("ps", 5)])
                    TT("dve", rcp[64:65, :].rearrange("p (j q) -> p j q", j=4),
                       pso[64:65, :].rearrange("p (j q) -> p j q", j=4),
                       esink[64:65, 4 * kv:4 * kv + 4].unsqueeze(2).to_broadcast([1, 4, 128]), ALU.add,
                       [("ps", 5), "esink"], ["rcp"])
                    P.op("dve", lambda e: e.reciprocal(rcp[64:65, :], rcp[64:65, :]), ["rcp"], ["rcp"])
                    MM(PS[7][0:64, :], onesM[64:65, 0:64], rcp[64:65, :], True, True, ["onesM", "rcp"], [("ps", 7)])
                    CP("dve", bcs[0:64, :], PS[7][0:64, :], [("ps", 7)], ["bcs"])
                    if kv == 0:
                        TT("dve", OAT[0:64, :, nq * 128:(nq + 1) * 128], pso[0:64, :].rearrange("p (j q) -> p j q", j=4),
                           bcs[0:64, :].rearrange("p (j q) -> p j q", j=4), ALU.mult, [("ps", 5), "bcs"], ["OAT"])
                    else:
                        TT("dve", tmpO[0:64, :], pso[0:64, :], bcs[0:64, :], ALU.mult, [("ps", 5), "bcs"], ["tmpO"])
                        P.dma("pool", lambda e, nq=nq: e.dma_start(
                            out=OAT[64:128, :, nq * 128:(nq + 1) * 128],
                            in_=tmpO[0:64, :].rearrange("p (j q) -> p j q", j=4)), ["tmpO"], ["OAT"])

                units = [(nq, kv) for nq in range(4) for kv in range(2)]
                a_stage_x(units[0][0], units[0][1], bufsets[0])
                for ui, (nq, kv) in enumerate(units):
                    if ui + 1 < len(units):
                        a_stage_x(units[ui + 1][0], units[ui + 1][1], bufsets[(ui + 1) % 2])
                    a_stage_y(nq, kv, bufsets[ui % 2])

                if stop == 'A':
                    return
                extB = [(expb[:, 0, :], ("expb", 0)), (expb[:, 1, :], ("expb", 1)), (expb[:, 2, :], ("expb", 2))]
                for kv in range(2):
                    pb = slice(kv * 64, (kv + 1) * 64)
                    for j in range(4):
                        pso = PS[5 + (j % 2)]

                        def b_qk(c, j=j, pb=pb):
                            bk = 2 + (c % 3)
                            MM(PS[bk][:], kBT[pb, c * 128:(c + 1) * 128], qBT[pb, j, :], True, True, ["kBT", "qBT"], [("ps", bk)])
                            ACTF(extB[c % 3][0], PS[bk][:], AF.Exp, [("ps", bk)], [extB[c % 3][1]], scale=0.125)

                        b_qk(0)
                        b_qk(1)
                        for c in range(NCH):
                            if c + 2 < NCH:
                                b_qk(c + 2)
                            MM(pso[0:65, :], vBa[:, c, kv, 0:65], extB[c % 3][0], c == 0, c == NCH - 1,
                               ["vB", "vB_ones", extB[c % 3][1]], [("ps", 5 + (j % 2))])
                        P.op("dve", lambda e, pso=pso: e.reciprocal(rcp[64:65, :], pso[64:65, :]), [("ps", 5 + (j % 2))], ["rcp"])
                        MM(PS[7][0:64, :], onesM[64:65, 0:64], rcp[64:65, :], True, True, ["onesM", "rcp"], [("ps", 7)])
                        CP("dve", bcs[0:64, :], PS[7][0:64, :], [("ps", 7)], ["bcs"])
                        if kv == 0:
                            TT("dve", OBT[0:64, j, :], pso[0:64, :], bcs[0:64, :], ALU.mult, [("ps", 5 + (j % 2)), "bcs"], ["OBT"])
                        else:
                            TT("dve", tmpO[0:64, :], pso[0:64, :], bcs[0:64, :], ALU.mult, [("ps", 5 + (j % 2)), "bcs"], ["tmpO"])
                            P.dma("pool", lambda e, j=j: e.dma_start(out=OBT[64:128, j, :], in_=tmpO[0:64, :]), ["tmpO"], ["OBT"])

                if stop == 'B':
                    return
                u, ut = load_unit(wba_d[:, 0:512].rearrange("(k p) n -> p k n", p=128), 4)
                u2, ut2 = load_unit(wba_d[:, 512:1024].rearrange("(k p) n -> p k n", p=128), 4)
                for m in range(8):
                    uu, uut = (u, ut) if m < 4 else (u2, ut2)
                    psm = PS[m % 2]
                    for k in range(4):
                        MM(psm[:], uu[:, k, (m % 4) * 128:(m % 4 + 1) * 128], OAT[:, k, :], k == 0, k == 3, [uut, "OAT"], [("ps", m % 2)])
                    CP("act", mrg[:, m, :], psm[:], [("ps", m % 2)], [("mrg", m), "qAT" if m < 4 else "qBT"])
                for hf in range(2):
                    u, ut = load_unit(wunit(win_d, 1536 + hf * 512))
                    for mm_ in range(4):
                        m = hf * 4 + mm_
                        psm = PS[m % 2]
                        for k in range(8):
                            MM(psm[:], u[:, k, mm_ * 128:(mm_ + 1) * 128], h0Tg[:, k, :], k == 0, k == 7, [ut, "h0Tg"], [("ps", m % 2)])
                        ACTF(t_sig, psm[:], AF.Sigmoid, [("ps", m % 2)], ["t_sig"])
                        TT("dve", mrg[:, m, :], mrg[:, m, :].bitcast(F32), t_sig, ALU.mult, [("mrg", m), "t_sig"], [("mrg", m)])
                for hf in range(2):
                    u, ut = load_unit(wbb_d[:, hf * 512:(hf + 1) * 512].rearrange("(k p) n -> p k n", p=128), 4)
                    u2, ut2 = load_unit(wunit(win_d, 2560 + hf * 512))
                    for mm_ in range(4):
                        m = hf * 4 + mm_
                        for k in range(4):
                            MM(PS[0][:], u[:, k, mm_ * 128:(mm_ + 1) * 128], OBT[:, k, :], k == 0, k == 3, [ut, "OBT"], [("ps", 0)])
                        for k in range(8):
                            MM(PS[1][:], u2[:, k, mm_ * 128:(mm_ + 1) * 128], h0Tg[:, k, :], k == 0, k == 7, [ut2, "h0Tg"], [("ps", 1)])
                        ACTF(t_sig, PS[1][:], AF.Sigmoid, [("ps", 1)], ["t_sig"])
                        TT("dve", t_a, PS[0][:], t_sig, ALU.mult, [("ps", 0), "t_sig"], ["t_a"])
                        TT("dve", mrg[:, m, :], mrg[:, m, :].bitcast(F32), t_a, ALU.add, [("mrg", m), "t_a"], [("mrg", m)])
                if stop == 'M':
                    return
                uo = []
                for hf in range(2):
                    uo.append(load_unit(wunit(wout_d, hf * 512)))
                mtoks = [("mrg", m) for m in range(8)]
                for j in range(4):
                    c = g * 4 + j
                    h0t = wk[:, j, :]
                    DMA("sp", h0t, H0_d[c * 128:(c + 1) * 128, :], [("H0", c)], [("wk", j)])
                    for hf in range(2):
                        u, ut = uo[hf]
                        psw = PS[hf]
                        for k in range(8):
                            MM(psw[:], mrg[:, k, j * 128:(j + 1) * 128], u[:, k, :], k == 0, k == 7, [ut] + mtoks, [("ps", hf)])
                        STT("dve", h0t[:, hf * 512:(hf + 1) * 512], h0t[:, hf * 512:(hf + 1) * 512], ALPHA, psw[:],
                            ALU.mult, ALU.add, [("wk", j), ("ps", hf)], [("wk", j)])
                    layer_norm(h0t, h0t, h0t, [("wk", j)], [("wk", j)], "ln1")
                    DMA("act", H1_d[c * 128:(c + 1) * 128, :], h0t, [("wk", j)], [("H1", c)])
                    if stage == 1:
                        DMA("act", dbg_d[c * 128:(c + 1) * 128, :], h0t, [("wk", j)], [("dbg", c)])


        if stop not in ("p1", "c0", "ln", "tr", "k", "kr", "v"):
            phase2()
        outs = []
        if stage >= 2:
            P.barrier()
            DMA("sp", wrt[:], wr_d.rearrange("(k p) n -> p k n", p=128), [], ["wrt"])
            DMA("sp", bdn[:], bd_d, [], ["bdn"])
            bgs = wk[0:32, 2, :]
            bus = wk[0:32, 3, :]
            DMA("sp", bgs, bg_d, [], ["bgs"])
            DMA("sp", bus, bu_d, [], ["bus"])
            for (src, stok, dstT, dtok) in ((bgs, "bgs", bgT, "bgT"), (bus, "bus", buT, "buT")):
                pst = PS[0]
                for m in range(8):
                    TR(pst[:, m * 32:(m + 1) * 32], src[:, m * 128:(m + 1) * 128], ident[0:32, 0:32], [stok], [("ps", 0)])
                CP("dve", dstT[:], pst[:, 0:256].rearrange("p (m e) -> p m e", m=8), [("ps", 0)], [dtok])
            P.op("pool", lambda e: e.memset(msum[:], 0.0), [], ["msum"])
            brt = smallb[:, 136:168]
            for c in range(NCH):
                h1t = wk[:, c % 2, :]
                htok = ("wk", c % 2)
                DMA("sp", h1t, H1_d[c * 128:(c + 1) * 128, :], [], [htok])
                for half in range(2):
                    pst = PS[2 + half]
                    for kk in range(4):
                        k = half * 4 + kk
                        TR(pst[:, kk * 128:(kk + 1) * 128], h1t[:, k * 128:(k + 1) * 128], ident[:], [htok], [("ps", 2 + half)])
                    CP("act" if half == 0 else "dve", (rcp if half == 0 else bcs)[:], pst[:], [("ps", 2 + half)],
                       ["h1T%d" % half])
                psl = PS[4]
                for k in range(8):
                    srcT = (rcp if k < 4 else bcs)[:, (k % 4) * 128:(k % 4 + 1) * 128]
                    MM(psl[:, 0:32], srcT, wrt[:, k, :], k == 0, k == 7, ["h1T0", "h1T1", "wrt"], [("ps", 4)])
                lg = rt[:, 0:32]
                TT("dve", lg, psl[:, 0:32], brt, ALU.add, [("ps", 4), "smallb"], ["lg"])
                mx = rt[:, 32:40]
                P.op("dve", lambda e, mx=mx, lg=lg: e.max(mx, lg), ["lg"], ["mx"])
                P.op("dve", lambda e, mx=mx, lg=lg: e.max_index(rti[:], mx, lg), ["lg", "mx"], ["rti"])
                negm = rt[:, 40:41]
                TS("dve", negm, mx[:, 0:1], -1.0, None, ALU.mult, None, ["mx"], ["negm"])
                e4 = rt[:, 44:48]
                esum = rt[:, 48:49]
                ACTF(e4, mx[:, 0:4], AF.Exp, ["mx", "negm"], ["e4", "esum"], bias=negm, accum=esum)
                P.op("dve", lambda e, esum=esum: e.reciprocal(rt[:, 49:50], esum), ["esum"], ["ersum"])
                TS("dve", wts[:, c, :], e4, rt[:, 49:50], None, ALU.mult, None, ["e4", "ersum"], ["wts"])
                mk = rt[:, 64:96]
                TS("dve", mk, lg, mx[:, 3:4], None, ALU.is_ge, None, ["lg", "mx"], ["mk"])
                psr = PS[5]
                if c > 0:
                    MM(psr[:, 0:32], onesM[:], msum[:], True, False, ["onesM", "msum"], [("ps", 5)])
                MM(psr[:, 0:32], tri[:], mk, c == 0, True, ["tri", "mk"], [("ps", 5)])
                TT("pool", msum[:], msum[:], mk, ALU.add, ["msum", "mk"], ["msum"])
                idxf = rt[:, 96:100]
                CP("dve", idxf, rti[:, 0:4], ["rti"], ["idxf"])
                oh = rt[:, 128:256].rearrange("p (k e) -> p k e", k=4)
                TT("dve", oh, iota[:].unsqueeze(1).to_broadcast([128, 4, 32]),
                   idxf.unsqueeze(2).to_broadcast([128, 4, 32]), ALU.is_equal, ["iota", "idxf"], ["oh"])
                pr = rt[:, 256:384].rearrange("p (k e) -> p k e", k=4)
                TT("dve", pr, oh, psr[:, 0:32].unsqueeze(1).to_broadcast([128, 4, 32]), ALU.mult, ["oh", ("ps", 5)], ["pr"])
                rk = rt[:, 100:104]
                P.op("dve", lambda e, rk=rk, pr=pr: e.reduce_sum(rk, pr, AX.X), ["pr"], ["rk"])
                posf = rt[:, 104:108]
                STT("dve", posf, idxf, float(CAP), rk, ALU.mult, ALU.add, ["idxf", "rk"], ["posf"])
                CP("dve", posi[:, c, :], posf, ["posf"], ["posi"])
                pw = rt[:, 384:512].rearrange("p (k e) -> p k e", k=4)
                TT("dve", pw, oh, wts[:, c, :].unsqueeze(2).to_broadcast([128, 4, 32]), ALU.mult, ["oh", "wts"], ["pw"])
                wdn = rt[:, 512:544]
                P.op("dve", lambda e, wdn=wdn, pw=pw: e.reduce_sum(wdn, pw.rearrange("p k e -> p e k"), AX.X), ["pw"], ["wdn"])
                TR(PS[6][0:32, 0:128], wdn, ident[:], ["wdn"], [("ps", 6)])
                CP("act", WdT[:, c * 128:(c + 1) * 128], PS[6][0:32, 0:128], [("ps", 6)], ["WdT"])
                for k4 in range(4):
                    P.dma("pool", lambda e, c=c, k4=k4, h1t=h1t: e.indirect_dma_start(
                        out=XS_d, out_offset=bass.IndirectOffsetOnAxis(ap=posi[:, c, k4:k4 + 1], axis=0),
                        in_=h1t, in_offset=None), [htok, "posi"], [("XS", c, k4)])

            P.barrier()
            ring5 = [UA[:, i * 4096:(i + 1) * 4096].rearrange("p (k n) -> p k n", k=8) for i in range(5)]
            rs5 = {"n": 0}

            def load5(src_ap):
                i = rs5["n"] % 5
                rs5["n"] += 1
                DMA("sp", ring5[i], src_ap, [], [("r5", i)])
                return ring5[i], ("r5", i)

            def eunit(w_ap, e, hf):
                return w_ap[e, :, hf * 512:(hf + 1) * 512].rearrange("(k p) n -> p k n", p=128)

            gTb = [gT, rcp[:, 0:CAP]]
            sgTb = [sgT, bcs[:, 0:CAP]]
            uTb = [uT, rt[:, 0:CAP]]
            t1Tb = [t1T, UC[:, 6656:6656 + CAP]]
            for e_ in range(NE):
                if e_ == 0:
                    DMA("act", Xe, XS_d[0:CAP, :].rearrange("(j p) d -> p j d", p=128), [], ["Xe"])
                ug = [None, None]
                uu_ = [None, None]
                ug[0] = load5(eunit(wg_d, e_, 0))
                uu_[0] = load5(eunit(wu_d, e_, 0))
                for k in range(8):
                    pst = PS[6 + (k % 2)]
                    for j in range(3):
                        TR(pst[:, j * 128:(j + 1) * 128], Xe[:, j, k * 128:(k + 1) * 128], ident[:], ["Xe"], [("ps", 6 + (k % 2))])
                    CP("act" if k % 2 == 0 else "dve", xTe[:, k, :], pst[:, 0:384], [("ps", 6 + (k % 2))], ["xTe"])
                if e_ + 1 < NE:
                    DMA("act", Xe, XS_d[(e_ + 1) * CAP:(e_ + 2) * CAP, :].rearrange("(j p) d -> p j d", p=128), [], ["Xe"])
                ug[1] = load5(eunit(wg_d, e_, 1))
                uu_[1] = load5(eunit(wu_d, e_, 1))
                for m in range(8):
                    hf, mm_ = m // 4, m % 4
                    (gu, gt), (uu, utk) = ug[hf], uu_[hf]
                    psg = PS[(m % 2) * 2]
                    psu = PS[(m % 2) * 2 + 1]
                    for k in range(8):
                        MM(psg[:, 0:CAP], gu[:, k, mm_ * 128:(mm_ + 1) * 128], xTe[:, k, :], k == 0, k == 7, [gt, "xTe"], [("ps", (m % 2) * 2)])
                    for k in range(8):
                        MM(psu[:, 0:CAP], uu[:, k, mm_ * 128:(mm_ + 1) * 128], xTe[:, k, :], k == 0, k == 7, [utk, "xTe"], [("ps", (m % 2) * 2 + 1)])
                    pb_ = m % 2
                    gT_, sgT_, uT_, t1T_ = gTb[pb_], sgTb[pb_], uTb[pb_], t1Tb[pb_]
                    TS("dve", gT_, psg[:, 0:CAP], bgT[:, m, e_:e_ + 1], 7.0, ALU.add, ALU.min, [("ps", (m % 2) * 2), "bgT"], [("gT", pb_)])
                    ACTF(sgT_, gT_, AF.Sigmoid, [("gT", pb_)], [("sgT", pb_)], scale=1.702)
                    TS("dve", uT_, psu[:, 0:CAP], buT[:, m, e_:e_ + 1], 7.0, ALU.add, ALU.min, [("ps", (m % 2) * 2 + 1), "buT"], [("uT", pb_)])
                    TS("dve", uT_, uT_, -7.0, 1.0, ALU.max, ALU.add, [("uT", pb_)], [("uT", pb_)])
                    TT("dve", t1T_, gT_, sgT_, ALU.mult, [("gT", pb_), ("sgT", pb_)], [("t1T", pb_)])
                    TT("dve", actT[:, m, :], t1T_, uT_, ALU.mult, [("t1T", pb_), ("uT", pb_)], ["actT"])
                ud = [load5(eunit(wd_d, e_, 0)), load5(eunit(wd_d, e_, 1))]
                for t in range(3):
                    yt = ytile[t % 2]
                    ytok = ("yt", t % 2)
                    for hf in range(2):
                        u, ut = ud[hf]
                        psy = PS[4 + hf]
                        for k in range(8):
                            MM(psy[:], actT[:, k, t * 128:(t + 1) * 128], u[:, k, :], k == 0, k == 7, [ut, "actT"], [("ps", 4 + hf)])
                        CP("act", yt[:, hf * 512:(hf + 1) * 512], psy[:], [("ps", 4 + hf)], [ytok])
                    DMA("act", YS_d[e_ * CAP + t * 128:e_ * CAP + (t + 1) * 128, :], yt, [ytok], [("YS", e_, t)])

            P.barrier()
            DMA("sp", lnG[:], lnp_d[4:5, :].to_broadcast([128, D]), [], ["lnG"])
            DMA("sp", lnB[:], lnp_d[5:6, :].to_broadcast([128, D]), [], ["lnB"])
            gsets = [
                [UC[:, 0:1024], UC[:, 1024:2048], UC[:, 2048:3072], UC[:, 3072:4096]],
                [UC[:, 4096:5120], UC[:, 5120:6144], UC[:, 6144:7168], wk[:, 2, :]],
            ]
            for c in range(NCH):
                par = c % 2
                h1t = wk[:, par, :]
                htok = ("wk", par)
                DMA("sp", h1t, H1_d[c * 128:(c + 1) * 128, :], [], [htok])
                gts = []
                for k4 in range(4):
                    gt_ = gsets[par][k4]
                    gk_ = ("ga", par, k4)
                    P.dma("pool", lambda e, c=c, k4=k4, gt_=gt_: e.indirect_dma_start(
                        out=gt_, out_offset=None, in_=YS_d,
                        in_offset=bass.IndirectOffsetOnAxis(ap=posi[:, c, k4:k4 + 1], axis=0)), ["posi"], [gk_])
                    gts.append((gt_, gk_))
                for hf in range(2):
                    MM(PS[2 * par + hf][:], WdT[:, c * 128:(c + 1) * 128], bdn[:, hf * 512:(hf + 1) * 512], True, True,
                       ["WdT", "bdn"], [("ps", 2 * par + hf)])
                acc, atok = gts[0]
                TS("dve", acc, acc, wts[:, c, 0:1], None, ALU.mult, None, [atok, "wts"], [atok])
                for k4 in range(1, 4):
                    STT("dve", acc, gts[k4][0], wts[:, c, k4:k4 + 1], acc, ALU.mult, ALU.add,
                        [gts[k4][1], "wts", atok], [atok])
                for hf in range(2):
                    TT("dve", acc[:, hf * 512:(hf + 1) * 512], acc[:, hf * 512:(hf + 1) * 512], PS[2 * par + hf][:], ALU.add,
                       [atok, ("ps", 2 * par + hf)], [atok])
                STT("dve", acc, h1t, ALPHA, acc, ALU.mult, ALU.add, [htok, atok], [atok])
                layer_norm(acc, acc, acc, [atok], [atok], "ln2")
                outs.append(DMA("act", out_d[c * 128:(c + 1) * 128, :], acc, [atok], [("out", c)]))
        if stage < 3:
            fw = {"act": list(P.dma_ops["act"][-10:]), "sp": list(P.dma_ops["sp"][-10:]), "pool": list(P.dma_ops["pool"][-10:])}
        else:
            fw = {"act": outs}
        P.emit(fw)
    return nc


def _prep_inputs(inp):
    f = lambda a: np.ascontiguousarray(np.asarray(a, dtype=np.float32))
    w_in = f(inp["w_in"])[0]
    qa, ka, va, qg, kg, vg, ga, gb = np.split(w_in, np.cumsum([512, 128, 128, 512, 128, 128, 1024])[:], axis=1)

    def qperm(q):
        cols = []
        for j in range(4):
            cols.append(q[:, j * 64:(j + 1) * 64])
            cols.append(q[:, (j + 4) * 64:(j + 5) * 64])
        return np.concatenate(cols, axis=1)

    w_inp = np.ascontiguousarray(np.concatenate([qperm(qa), qperm(qg), ka, kg, va, vg, ga, gb], axis=1))

    def bperm(w):
        rows = []
        for j in range(4):
            rows.append(w[j * 64:(j + 1) * 64])
            rows.append(w[(j + 4) * 64:(j + 5) * 64])
        return np.ascontiguousarray(np.concatenate(rows, axis=0))

    lnp = np.stack([f(inp["ln0_g"]), f(inp["ln0_b"]), f(inp["ln1_g"])[0], f(inp["ln1_b"])[0],
                    f(inp["ln2_g"])[0], f(inp["ln2_b"])[0]], axis=0)
    small = np.concatenate([f(inp["a_sink"])[0], f(inp["b_q_norm"])[0], f(inp["b_k_norm"])[0],
                            f(inp["b_router"])[0]])[None, :]
    shared = {
        "lnp": np.ascontiguousarray(lnp), "w_in": w_inp, "small": np.ascontiguousarray(small),
        "w_ba": bperm(f(inp["w_branch_a"])[0]), "w_bb": bperm(f(inp["w_branch_b"])[0]),
        "w_out": f(inp["w_out"])[0], "w_router": f(inp["w_router"])[0],
        "w_gate": f(inp["w_gate"])[0], "w_up": f(inp["w_up"])[0], "w_down": f(inp["w_down"])[0],
        "b_gate": f(inp["b_gate"])[0], "b_up": f(inp["b_up"])[0], "b_down": f(inp["b_down"])[0],
    }
    shared.update(_consts())
    return shared


def kernel(**inputs):
    shared = _prep_inputs(inputs)
    x = np.asarray(inputs["x"], dtype=np.float32)
    n = x.shape[0]
    nc = build_nc(3)
    in_maps = []
    for b in range(n):
        m = dict(shared)
        m["x"] = np.ascontiguousarray(x[b])
        in_maps.append(m)
    res = run_bass_kernel_spmd(nc, in_maps, core_ids=list(range(n)))
    return np.stack([np.asarray(r["out"], dtype=np.float32) for r in res.results], axis=0)
```

```python
import contextlib
import os
import numpy as np
import concourse.bass as bass
import concourse.mybir as mybir
from concourse.bass_utils import run_bass_kernel_spmd

F32 = mybir.dt.float32
F32R = mybir.dt.float32r
U32 = mybir.dt.uint32
I32 = mybir.dt.int32
ALU = mybir.AluOpType
AF = mybir.ActivationFunctionType
AX = mybir.AxisListType

S = 2048
D = 1024
NCH = 16
NG = 4
NE = 32
CAP = 384
NSLOT = NE * CAP
ALPHA = 2.0 ** 0.25
LN_EPS = 1e-5
RMS_EPS = 1e-6
MASKV = -30000.0
ENGS = ["pe", "act", "dve", "pool", "sp"]


class Op:
    __slots__ = ("eng", "fn", "deps", "signal", "sig_idx", "is_dma", "dma_sem", "dma_val")

    def __init__(self, eng, fn, is_dma):
        self.eng = eng
        self.fn = fn
        self.deps = []
        self.signal = False
        self.sig_idx = None
        self.is_dma = is_dma
        self.dma_sem = None
        self.dma_val = None


class Prog:
    def __init__(self, nc, dma_ring=10):
        self.nc = nc
        self.ops = {e: [] for e in ENGS}
        self.tok = {}
        self.dma_ring = dma_ring
        self.dma_count = {e: 0 for e in ENGS}
        self.dma_ops = {e: [] for e in ENGS}

    def _track(self, o, reads, writes):
        for t in reads:
            st = self.tok.get(t)
            if st is None:
                st = self.tok[t] = [None, []]
            if st[0] is not None:
                o.deps.append((st[0], "raw"))
            st[1].append(o)
        for t in writes:
            st = self.tok.get(t)
            if st is None:
                st = self.tok[t] = [None, []]
            if st[0] is not None:
                o.deps.append((st[0], "waw"))
            for r in st[1]:
                if r is not o:
                    o.deps.append((r, "war"))
            st[0] = o
            st[1] = []

    def op(self, eng, fn, reads=(), writes=()):
        o = Op(eng, fn, False)
        self._track(o, reads, writes)
        self.ops[eng].append(o)
        return o

    def dma(self, eng, fn, reads=(), writes=()):
        o = Op(eng, fn, True)
        self._track(o, reads, writes)
        i = self.dma_count[eng]
        self.dma_count[eng] += 1
        o.dma_sem = (eng, i % self.dma_ring)
        o.dma_val = 16 * (i // self.dma_ring + 1)
        if i >= self.dma_ring:
            o.deps.append((self.dma_ops[eng][i - self.dma_ring], "ring"))
        self.dma_ops[eng].append(o)
        self.ops[eng].append(o)
        return o

    def barrier(self):
        lasts = []
        for e in ENGS:
            comp = [o for o in self.ops[e] if not o.is_dma and o.fn is not None]
            if comp:
                lasts.append(comp[-1])
            lasts.extend(self.dma_ops[e][-self.dma_ring:])
        for e in ENGS:
            o = Op(e, None, False)
            for l in lasts:
                o.deps.append((l, "bar"))
            self.ops[e].append(o)
        self.tok = {}

    @staticmethod
    def _skip(o, d, kind):
        if d.is_dma:
            return False
        if d.eng == o.eng and not o.is_dma and kind != "bar":
            if d.eng == "pe" or kind != "raw":
                return True
        return False

    def emit(self, final_wait):
        nc = self.nc
        for e in ENGS:
            for o in self.ops[e]:
                for d, kind in o.deps:
                    if d.is_dma or self._skip(o, d, kind):
                        continue
                    d.signal = True
        for e in ENGS:
            c = 0
            for o in self.ops[e]:
                if o.signal:
                    c += 1
                    o.sig_idx = c
        with contextlib.ExitStack() as st:
            st.enter_context(nc.cleanup_on_exit())
            esem = {e: nc.alloc_semaphore(name="s_" + e) for e in ENGS}
            dsem = {}
            for e in ENGS:
                for i in range(min(self.dma_ring, self.dma_count[e])):
                    dsem[(e, i)] = nc.alloc_semaphore(name="d_%s_%d" % (e, i))
            for sm in list(esem.values()) + list(dsem.values()):
                nc.gpsimd.sem_clear(sm)
            block = st.enter_context(nc.Block())

            def run(e, eng):
                seen = {}
                for o in self.ops[e]:
                    need = {}
                    for d, kind in o.deps:
                        if self._skip(o, d, kind):
                            continue
                        if d.is_dma:
                            key, sem, val = ("d",) + d.dma_sem, dsem[d.dma_sem], d.dma_val
                        else:
                            key, sem, val = ("e", d.eng), esem[d.eng], d.sig_idx
                        if seen.get(key, 0) >= val:
                            continue
                        if key not in need or need[key][1] < val:
                            need[key] = (sem, val)
                    for key, (sem, val) in need.items():
                        eng.wait_ge(sem, val)
                        seen[key] = val
                    if o.fn is None:
                        continue
                    ins = o.fn(eng)
                    if o.is_dma:
                        ins.then_inc(dsem[o.dma_sem], 16)
                    elif o.signal:
                        ins.then_inc(esem[e], 1)
                for d in final_wait.get(e, []):
                    eng.wait_ge(dsem[d.dma_sem], d.dma_val)

            block.tensor(lambda eng: run("pe", eng))
            block.scalar(lambda eng: run("act", eng))
            block.vector(lambda eng: run("dve", eng))
            block.gpsimd(lambda eng: run("pool", eng))
            block.sync(lambda eng: run("sp", eng))


def _consts():
    c = {}
    c["ident"] = np.eye(128, dtype=np.float32)
    bo = np.zeros((128, 128), np.float32)
    bo[:64, :64] = 1.0 / 64
    bo[64:, 64:] = 1.0 / 64
    c["blockones"] = bo
    rot = np.zeros((128, 128), np.float32)
    for m in range(128):
        r = (m % 64) % 32
        if r < 16:
            rot[m + 16, m] = -1.0
        else:
            rot[m - 16, m] = 1.0
    c["rot"] = rot
    t = np.arange(S)
    row = (t // 64).astype(np.float32)
    col = (t % 64).astype(np.float32)
    inv = (np.float32(10000.0) ** (-np.arange(16, dtype=np.float32) * np.float32(2.0 / 32))).astype(np.float32)
    cos = np.zeros((128, S), np.float32)
    sin = np.zeros((128, S), np.float32)
    for p in range(128):
        f = p % 64
        pos = row if f < 32 else col
        ang = (pos * inv[f % 16]).astype(np.float32)
        cos[p] = np.cos(ang)
        sin[p] = np.sin(ang)
    c["rcos"] = cos
    c["rsin"] = sin
    slopes = np.array([2.0 ** (-8.0 * (h + 1) / 8) for h in range(8)], np.float32)
    bias = np.zeros((128, 3, 2, 4, 128), np.float32)
    ki = np.arange(128)[:, None]
    qi = np.arange(128)[None, :]
    for oi, o in enumerate((-1, 0, 1)):
        dist = np.abs(qi - ki - 128 * o)
        for kv in range(2):
            for j in range(4):
                h = j + 4 * kv
                bias[:, oi, kv, j, :] = np.where(dist <= 128, -slopes[h] * dist.astype(np.float32), MASKV)
    c["abias"] = bias.reshape(128, 6 * 512)
    tri = (np.arange(128)[:, None] < np.arange(128)[None, :]).astype(np.float32)
    c["tri"] = tri
    c["iota32"] = np.tile(np.arange(32, dtype=np.float32)[None, :], (128, 1))
    return c


def build_nc(stage=3, stop=None):
    nc = bass.Bass("TRN2", target_bir_lowering=False)
    nc.dge_precook = False

    def din(name, shape, dt=F32):
        return nc.dram_tensor(name, list(shape), dt, kind="ExternalInput").ap()

    x_d = din("x", [S, D])
    lnp_d = din("lnp", [6, D])
    win_d = din("w_in", [D, 3584], F32R)
    small_d = din("small", [1, 8 + 64 + 64 + 32])
    wba_d = din("w_ba", [512, D], F32R)
    wbb_d = din("w_bb", [512, D], F32R)
    wout_d = din("w_out", [D, D], F32R)
    wr_d = din("w_router", [D, NE])
    wg_d = din("w_gate", [NE, D, D], F32R)
    wu_d = din("w_up", [NE, D, D], F32R)
    wd_d = din("w_down", [NE, D, D], F32R)
    bg_d = din("b_gate", [NE, D])
    bu_d = din("b_up", [NE, D])
    bd_d = din("b_down", [NE, D])
    c_ident = din("ident", [128, 128])
    c_bo = din("blockones", [128, 128], F32R)
    c_rot = din("rot", [128, 128], F32R)
    c_cos = din("rcos", [128, S])
    c_sin = din("rsin", [128, S])
    c_abias = din("abias", [128, 3072])
    c_tri = din("tri", [128, 128])
    c_iota = din("iota32", [128, 32])
    out_d = nc.dram_tensor("out", [S, D], F32, kind="ExternalOutput").ap()
    dbg_d = None
    if stage < 3:
        dbg_d = nc.dram_tensor("dbg", [S, D], F32, kind="ExternalOutput").ap()
    H0_d = nc.dram_tensor("H0", [S, D], F32, kind="Internal").ap()
    H0T_d = nc.dram_tensor("H0T", [NG, 128, 8 * 512], F32R, kind="Internal").ap()
    H1_d = nc.dram_tensor("H1", [S, D], F32, kind="Internal").ap()
    XS_d = nc.dram_tensor("XS", [NSLOT, D], F32, kind="Internal").ap()
    YS_d = nc.dram_tensor("YS", [NSLOT, D], F32, kind="Internal").ap()

    with contextlib.ExitStack() as st:
        def sb(name, shape, dt=F32):
            return st.enter_context(nc.sbuf_tensor(name, list(shape), dt))

        def psb(name):
            return st.enter_context(nc.psum_tensor(name, [128, 512], F32))

        UA = sb("UA", [128, 20480], F32R)
        UB = sb("UB", [128, 8320], F32R)
        expb = sb("expb", [128, 3, 512], F32R)
        tmpR = sb("tmpR", [128, 2, 512], F32R)
        tmpO = sb("tmpO", [128, 512], F32R)
        expb2 = sb("expb2", [128, 512], F32R)
        cbo = sb("cbo", [128, 128], F32R)
        crot = sb("crot", [128, 128], F32R)
        UC = sb("UC", [128, 7168], F32)
        wk = sb("wk", [128, 4, 1024], F32)
        lnG = sb("lnG", [128, 1024], F32)
        lnB = sb("lnB", [128, 1024], F32)
        ident = sb("identt", [128, 128], F32)
        tri = sb("trit", [128, 128], F32)
        onesM = sb("onesM", [128, 128], F32)
        iota = sb("iotat", [128, 32], F32)
        smallb = sb("smallb", [128, 168], F32)
        esink = sb("esinkt", [128, 8], F32)
        gqk = sb("gqk", [128, 2], F32)
        stats = sb("stats", [128, 2, 6], F32)
        mv = sb("mvt", [128, 2], F32)
        lnt = sb("lnt", [128, 4], F32)
        rcp = sb("rcp", [128, 512], F32)
        bcs = sb("bcs", [128, 512], F32)
        WdT = sb("WdT", [32, 2048], F32)
        bdn = sb("bdn", [32, 1024], F32)
        bgT = sb("bgT", [128, 8, 32], F32)
        buT = sb("buT", [128, 8, 32], F32)
        wrt = sb("wrt", [128, 8, 32], F32)
        rt = sb("rt", [128, 640], F32)
        rti = sb("rti", [128, 8], U32)
        posi = sb("posi", [128, NCH, 4], I32)
        wts = sb("wts", [128, NCH, 4], F32)
        msum = sb("msum", [128, 32], F32)
        PS = [psb("ps%d" % i) for i in range(8)]

        P = Prog(nc)
        print("sbuf bytes remaining", nc.sbuf_bytes_remaining)

        def MM(out, lhsT, rhs, start, stop, r, w):
            P.op("pe", lambda e: e.matmul(out, lhsT, rhs, start=start, stop=stop), r, w)

        def TR(out, in_, idn, r, w):
            P.op("pe", lambda e: e.transpose(out, in_, idn), list(r) + ["ident"], w)

        def ACTF(out, in_, func, r, w, bias=0.0, scale=1.0, accum=None):
            if accum is None:
                P.op("act", lambda e: e.activation(out, in_, func, bias=bias, scale=scale), r, w)
            else:
                P.op("act", lambda e: e.activation(out, in_, func, bias=bias, scale=scale, accum_out=accum), r, w)

        def TT(eng, out, a, b, op, r, w):
            P.op(eng, lambda e: e.tensor_tensor(out, a, b, op), r, w)

        def TS(eng, out, a, s1, s2, op0, op1, r, w):
            if s2 is None:
                P.op(eng, lambda e: e.tensor_scalar(out, a, s1, None, op0), r, w)
            else:
                P.op(eng, lambda e: e.tensor_scalar(out, a, s1, s2, op0, op1), r, w)

        def STT(eng, out, in0, scalar, in1, op0, op1, r, w):
            P.op(eng, lambda e: e.scalar_tensor_tensor(out, in0, scalar, in1, op0, op1), r, w)

        def CP(eng, out, in_, r, w):
            if eng == "act":
                P.op("act", lambda e: e.copy(out, in_), r, w)
            else:
                P.op(eng, lambda e: e.tensor_copy(out, in_), r, w)

        def DMA(q, out, in_, r, w):
            return P.dma(q, lambda e: e.dma_start(out=out, in_=in_), r, w)

        def layer_norm(src, dst, tmp, r, w, tag):
            P.op("dve", lambda e: e.bn_stats(stats[:, 0, :], src[:, 0:512]), r, ["st0"])
            P.op("dve", lambda e: e.bn_stats(stats[:, 1, :], src[:, 512:1024]), r, ["st1"])
            P.op("dve", lambda e: e.bn_aggr(mv[:], stats[:]), ["st0", "st1"], ["mv"])
            ACTF(lnt[:, 0:1], mv[:, 1:2], AF.Sqrt, ["mv"], ["ln_sd"], bias=LN_EPS)
            P.op("dve", lambda e: e.reciprocal(lnt[:, 1:2], lnt[:, 0:1]), ["ln_sd"], ["ln_rstd"])
            STT("dve", tmp, src, mv[:, 0:1], lnG[:], ALU.subtract, ALU.mult, list(r) + ["mv", "lnG"], [tag + "_t"])
            STT("dve", dst, tmp, lnt[:, 1:2], lnB[:], ALU.mult, ALU.add, [tag + "_t", "ln_rstd", "lnB"], w)

        ring = [UA[:, i * 4096:(i + 1) * 4096].rearrange("p (k n) -> p k n", k=8) for i in range(5)]
        h0Tg = UA[:, 8192:12288].rearrange("p (k n) -> p k n", k=8)
        qAT = UA[:, 12288:14336].rearrange("p (j n) -> p j n", j=4)
        qBT = UA[:, 14336:16384].rearrange("p (j n) -> p j n", j=4)
        OAT = UA[:, 16384:18432].rearrange("p (j n) -> p j n", j=4)
        OBT = UA[:, 18432:20480].rearrange("p (j n) -> p j n", j=4)
        mrg = UA[:, 12288:16384].rearrange("p (k n) -> p k n", k=8)
        kAT = UB[:, 0:2048]
        kBT = UB[:, 2048:4096]
        vAa = UB[:, 4096:6208].rearrange("p (c g d) -> p c g d", c=16, g=2)
        vBa = UB[:, 6208:8320].rearrange("p (c g d) -> p c g d", c=16, g=2)
        xTe = UB[:, 0:3072].rearrange("p (k n) -> p k n", k=8)
        actT = UB[:, 3072:6144].rearrange("p (k n) -> p k n", k=8)
        rcosg = UC[:, 0:512]
        rsing = UC[:, 512:1024]
        abias = UC[:, 1024:4096].rearrange("p (o n) -> p o n", o=6)
        t_rstd = UC[:, 4096:4608]
        t_a = UC[:, 4608:5120]
        t_b = UC[:, 5120:5632]
        t_sig = UC[:, 5632:6144]
        t_sp0 = UC[:, 6144:6656]
        t_sp1 = UC[:, 6656:7168]
        Xe = UC[:, 0:3072].rearrange("p (j d) -> p j d", j=3)
        gT = UC[:, 3072:3456]
        sgT = UC[:, 3456:3840]
        uT = UC[:, 3840:4224]
        t1T = UC[:, 4224:4608]
        ytile = [UC[:, 4608:5632], UC[:, 5632:6656]]

        DMA("sp", ident[:], c_ident, [], ["ident"])
        DMA("sp", cbo[:], c_bo, [], ["cbo"])
        DMA("sp", crot[:], c_rot, [], ["crot"])
        DMA("sp", tri[:], c_tri, [], ["tri"])
        DMA("sp", iota[:], c_iota, [], ["iota"])
        DMA("sp", smallb[:], small_d.to_broadcast([128, 168]), [], ["smallb"])
        DMA("sp", abias, c_abias.rearrange("p (o n) -> p o n", o=6), [], ["abias"])
        DMA("sp", lnG[:], lnp_d[0:1, :].to_broadcast([128, D]), [], ["lnG"])
        DMA("sp", lnB[:], lnp_d[1:2, :].to_broadcast([128, D]), [], ["lnB"])
        P.op("pool", lambda e: e.memset(onesM[:], 1.0), [], ["onesM"])
        ACTF(esink[:], smallb[:, 0:8], AF.Exp, ["smallb"], ["esink"])
        gsel = rt[:, 0:64]
        TT("dve", gsel, smallb[:, 8:72], ident[:, 0:64], ALU.mult, ["smallb", "ident"], ["gsel"])
        TT("dve", rt[:, 64:128], smallb[:, 8:72], ident[:, 64:128], ALU.mult, ["smallb", "ident"], ["gsel2"])
        TT("dve", gsel, gsel, rt[:, 64:128], ALU.add, ["gsel", "gsel2"], ["gsel3"])
        P.op("dve", lambda e: e.reduce_sum(gqk[:, 0:1], gsel, AX.X), ["gsel3"], ["gq"])
        gsel_k = rt[:, 128:192]
        TT("dve", gsel_k, smallb[:, 72:136], ident[:, 0:64], ALU.mult, ["smallb", "ident"], ["gselk"])
        TT("dve", rt[:, 192:256], smallb[:, 72:136], ident[:, 64:128], ALU.mult, ["smallb", "ident"], ["gselk2"])
        TT("dve", gsel_k, gsel_k, rt[:, 192:256], ALU.add, ["gselk", "gselk2"], ["gselk3"])
        P.op("dve", lambda e: e.reduce_sum(gqk[:, 1:2], gsel_k, AX.X), ["gselk3"], ["gk"])
        ones_v = onesM[:, 0:32].rearrange("p (c g d) -> p c g d", c=16, g=2)
        CP("dve", vAa[:, :, :, 64:65], ones_v, ["onesM"], ["vA_ones"])
        CP("dve", vBa[:, :, :, 64:65], ones_v, ["onesM"], ["vB_ones"])

        def rms_rope(ps_in, ps_tok, gcol, dst, dst_tok, bset=0):
            if bset == 0:
                sq, sqk, kn, knk = tmpR[:, 0, :], "tmpR0", tmpR[:, 1, :], "tmpR1"
                rs, rsk, ta, tak, tb, tbk = t_rstd, "t_rstd", t_a, "t_a", t_b, "t_b"
                p6, p7 = 6, 7
            else:
                sq, sqk, kn, knk = expb[:, 0, :], ("expb", 0), expb[:, 1, :], ("expb", 1)
                rs, rsk, ta, tak, tb, tbk = t_sig, "t_sig", t_sp0, "t_sp0", t_sp1, "t_sp1"
                p6, p7 = 2, 3
            ACTF(sq, ps_in, AF.Square, [ps_tok], [sqk])
            MM(PS[p6][:], cbo[:], sq, True, True, ["cbo", sqk], [("ps", p6)])
            ACTF(rs, PS[p6][:], AF.Sqrt, [("ps", p6)], [rsk], bias=RMS_EPS)
            P.op("dve", lambda e: e.reciprocal(rs, rs), [rsk], [rsk])
            STT("dve", kn, ps_in, gcol, rs, ALU.mult, ALU.mult, [ps_tok, rsk, "gq", "gk"], [knk])
            MM(PS[p7][:], crot[:], kn, True, True, ["crot", knk], [("ps", p7)])
            TT("dve", ta, kn.bitcast(F32), rcosg, ALU.mult, [knk, "rope"], [tak])
            TT("dve", tb, PS[p7][:], rsing, ALU.mult, [("ps", p7), "rope"], [tbk])
            TT("dve", dst, ta, tb, ALU.add, [tak, tbk], dst_tok)

        kv_unit = ring[0]
        p1_groups = NG if stop not in ("c0", "ln", "tr", "k", "kr", "v") else (0 if stop == "c0" else 1)
        DMA("sp", kv_unit, win_d[:, 1024:1536].rearrange("(k p) n -> p k n", p=128), [], [("ring", 0)])
        for g in range(p1_groups):
            DMA("sp", rcosg, c_cos[:, g * 512:(g + 1) * 512], [], ["rope"])
            DMA("sp", rsing, c_sin[:, g * 512:(g + 1) * 512], [], ["rope"])
            for j in range(4):
                c = g * 4 + j
                xt = wk[:, j, :]
                DMA("sp", xt, x_d[c * 128:(c + 1) * 128, :], [], [("wk", j)])
                layer_norm(xt, xt, xt, [("wk", j)], [("wk", j)], "ln0")
                DMA("act", H0_d[c * 128:(c + 1) * 128, :], xt, [("wk", j)], [("H0", c)])
                if stop == "ln":
                    continue
                for half in range(2):
                    pst = PS[2 + half]
                    for kk in range(4):
                        k = half * 4 + kk
                        TR(pst[:, kk * 128:(kk + 1) * 128], xt[:, k * 128:(k + 1) * 128], ident[:], [("wk", j)], [("ps", 2 + half)])
                    CP("act" if half == 0 else "dve",
                       h0Tg[:, half * 4:(half + 1) * 4, j * 128:(j + 1) * 128],
                       pst[:].rearrange("p (k n) -> p k n", k=4), [("ps", 2 + half)], ["h0Tg"])
            if stop == "ln":
                continue
            DMA("act", H0T_d[g], UA[:, 8192:12288], ["h0Tg"], [("H0T", g)])
            if stop == "tr":
                continue
            for blk in range(2):
                psk = PS[blk]
                for k in range(8):
                    MM(psk[:], kv_unit[:, k, blk * 128:(blk + 1) * 128], h0Tg[:, k, :], k == 0, k == 7,
                       [("ring", 0), "h0Tg"], [("ps", blk)])
            CP("act", kAT[:, g * 512:(g + 1) * 512], PS[0][:], [("ps", 0)], ["kAT"])
            if stop == "k":
                continue
            rms_rope(PS[1][:], ("ps", 1), gqk[:, 1:2], kBT[:, g * 512:(g + 1) * 512], ["kBT"])
            if stop == "kr":
                continue
            for j in range(4):
                c = g * 4 + j
                psv = PS[4 + (j % 2)]
                for k in range(8):
                    MM(psv[:, 0:256], h0Tg[:, k, j * 128:(j + 1) * 128], kv_unit[:, k, 256:512], k == 0, k == 7,
                       [("ring", 0), "h0Tg"], [("ps", 4 + (j % 2))])
                veng = "act" if j % 2 == 0 else "dve"
                CP(veng, vAa[:, c, :, 0:64], psv[:, 0:128].rearrange("p (g d) -> p g d", g=2),
                   [("ps", 4 + (j % 2))], ["vA"])
                CP(veng, vBa[:, c, :, 0:64], psv[:, 128:256].rearrange("p (g d) -> p g d", g=2),
                   [("ps", 4 + (j % 2))], ["vB"])

        DMA("sp", lnG[:], lnp_d[2:3, :].to_broadcast([128, D]), [], ["lnG"])
        DMA("sp", lnB[:], lnp_d[3:4, :].to_broadcast([128, D]), [], ["lnB"])
        rstate = {"n": 0}

        def load_unit(src_ap, kch=8):
            i = rstate["n"] % 2
            rstate["n"] += 1
            DMA("sp", ring[i][:, 0:kch, :], src_ap, [], [("ring", i)])
            return ring[i], ("ring", i)

        def wunit(w_ap, c0, kchunks=8):
            return w_ap[:, c0:c0 + 512].rearrange("(k p) n -> p k n", p=128)

        def phase2():
            for g in range(NG):
                DMA("sp", UA[:, 8192:12288], H0T_d[g], [("H0T", g)], ["h0Tg"])
                DMA("sp", rcosg, c_cos[:, g * 512:(g + 1) * 512], [], ["rope"])
                DMA("sp", rsing, c_sin[:, g * 512:(g + 1) * 512], [], ["rope"])
                u, ut = load_unit(wunit(win_d, 0))
                for j in range(4):
                    psq = PS[j % 2]
                    for k in range(8):
                        MM(psq[:], u[:, k, j * 128:(j + 1) * 128], h0Tg[:, k, :], k == 0, k == 7, [ut, "h0Tg"], [("ps", j % 2)])
                    CP("act", qAT[:, j, :], psq[:], [("ps", j % 2)], ["qAT", ("mrg", j)])
                u, ut = load_unit(wunit(win_d, 512))
                for j in range(4):
                    psq = PS[j % 2]
                    for k in range(8):
                        MM(psq[:], u[:, k, j * 128:(j + 1) * 128], h0Tg[:, k, :], k == 0, k == 7, [ut, "h0Tg"], [("ps", j % 2)])
                    rms_rope(psq[:], ("ps", j % 2), gqk[:, 0:1], qBT[:, j, :], ["qBT", ("mrg", 4 + j)], bset=j % 2)

                if stop == 'q':
                    return
                bufsets = [
                    dict(banks=[2, 3, 4], ex=[expb[:, 0, :], expb[:, 1, :], expb[:, 2, :]],
                         extok=[("expb", 0), ("expb", 1), ("expb", 2)],
                         ts=[t_a, t_b, t_sig], tstok=["t_a", "t_b", "t_sig"]),
                    dict(banks=[0, 1, 6], ex=[tmpR[:, 0, :], tmpR[:, 1, :], expb2[:]],
                         extok=["tmpR0", "tmpR1", ("expb", 5)],
                         ts=[t_rstd, t_sp0, t_sp1], tstok=["t_rstd", "t_sp0", "t_sp1"]),
                ]

                def a_stage_x(nq, kv, bs):
                    n = g * 4 + nq
                    offs = [o for o in (-1, 0, 1) if 0 <= n + o < NCH]
                    pb = slice(kv * 64, (kv + 1) * 64)
                    for oi, o in enumerate(offs):
                        c = n + o
                        bk = bs["banks"][oi]
                        pss = PS[bk]
                        for j in range(4):
                            MM(pss[:, j * 128:(j + 1) * 128], kAT[pb, c * 128:(c + 1) * 128],
                               qAT[pb, j, nq * 128:(nq + 1) * 128], True, True, ["kAT", "qAT"], [("ps", bk)])
                        STT("dve", bs["ts"][oi], pss[:], 0.125, abias[:, (o + 1) * 2 + kv, :], ALU.mult, ALU.add,
                            [("ps", bk), "abias"], [bs["tstok"][oi]])
                        ACTF(bs["ex"][oi], bs["ts"][oi], AF.Exp, [bs["tstok"][oi]], [bs["extok"][oi]])

                def a_stage_y(nq, kv, bs):
                    n = g * 4 + nq
                    offs = [o for o in (-1, 0, 1) if 0 <= n + o < NCH]
                    pso = PS[5]
                    for j in range(4):
                        for oi, o in enumerate(offs):
                            c = n + o
                            MM(pso[0:65, j * 128:(j + 1) * 128], vAa[:, c, kv, 0:65], bs["ex"][oi][:, j * 128:(j + 1) * 128],
                               oi == 0, oi == len(offs) - 1, ["vA", "vA_ones", bs["extok"][oi]], [("ps", 5)])
                    TT("dve", rcp[64:65, :].rearrange("p (j q) -> p j q", j=4),
                       pso[64:65, :].rearrange("p (j q) -> p j q", j=4),
                       esink[64:65, 4 * kv:4 * kv + 4].unsqueeze(2).to_broadcast([1, 4, 128]), ALU.add,
                       [("ps", 5), "esink"], ["rcp"])
                    P.op("dve", lambda e: e.reciprocal(rcp[64:65, :], rcp[64:65, :]), ["rcp"], ["rcp"])
                    MM(PS[7][0:64, :], onesM[64:65, 0:64], rcp[64:65, :], True, True, ["onesM", "rcp"], [("ps", 7)])
                    CP("dve", bcs[0:64, :], PS[7][0:64, :], [("ps", 7)], ["bcs"])
                    if kv == 0:
                        TT("dve", OAT[0:64, :, nq * 128:(nq + 1) * 128], pso[0:64, :].rearrange("p (j q) -> p j q", j=4),
                           bcs[0:64, :].rearrange("p (j q) -> p j q", j=4), ALU.mult, [("ps", 5), "bcs"], ["OAT"])
                    else:
                        TT("dve", tmpO[0:64, :], pso[0:64, :], bcs[0:64, :], ALU.mult, [("ps", 5), "bcs"], ["tmpO"])
                        P.dma("pool", lambda e, nq=nq: e.dma_start(
                            out=OAT[64:128, :, nq * 128:(nq + 1) * 128],
                            in_=tmpO[0:64, :].rearrange("p (j q) -> p j q", j=4)), ["tmpO"], ["OAT"])

                units = [(nq, kv) for nq in range(4) for kv in range(2)]
                a_stage_x(units[0][0], units[0][1], bufsets[0])
                for ui, (nq, kv) in enumerate(units):
                    if ui + 1 < len(units):
                        a_stage_x(units[ui + 1][0], units[ui + 1][1], bufsets[(ui + 1) % 2])
                    a_stage_y(nq, kv, bufsets[ui % 2])

                if stop == 'A':
                    return
                extB = [(expb[:, 0, :], ("expb", 0)), (expb[:, 1, :], ("expb", 1)), (expb[:, 2, :], ("expb", 2))]
                for kv in range(2):
                    pb = slice(kv * 64, (kv + 1) * 64)
                    for j in range(4):
                        pso = PS[5 + (j % 2)]

                        def b_qk(c, j=j, pb=pb):
                            bk = 2 + (c % 3)
                            MM(PS[bk][:], kBT[pb, c * 128:(c + 1) * 128], qBT[pb, j, :], True, True, ["kBT", "qBT"], [("ps", bk)])
                            ACTF(extB[c % 3][0], PS[bk][:], AF.Exp, [("ps", bk)], [extB[c % 3][1]], scale=0.125)

                        b_qk(0)
                        b_qk(1)
                        for c in range(NCH):
                            if c + 2 < NCH:
                                b_qk(c + 2)
                            MM(pso[0:65, :], vBa[:, c, kv, 0:65], extB[c % 3][0], c == 0, c == NCH - 1,
                               ["vB", "vB_ones", extB[c % 3][1]], [("ps", 5 + (j % 2))])
                        P.op("dve", lambda e, pso=pso: e.reciprocal(rcp[64:65, :], pso[64:65, :]), [("ps", 5 + (j % 2))], ["rcp"])
                        MM(PS[7][0:64, :], onesM[64:65, 0:64], rcp[64:65, :], True, True, ["onesM", "rcp"], [("ps", 7)])
                        CP("dve", bcs[0:64, :], PS[7][0:64, :], [("ps", 7)], ["bcs"])
                        if kv == 0:
                            TT("dve", OBT[0:64, j, :], pso[0:64, :], bcs[0:64, :], ALU.mult, [("ps", 5 + (j % 2)), "bcs"], ["OBT"])
                        else:
                            TT("dve", tmpO[0:64, :], pso[0:64, :], bcs[0:64, :], ALU.mult, [("ps", 5 + (j % 2)), "bcs"], ["tmpO"])
                            P.dma("pool", lambda e, j=j: e.dma_start(out=OBT[64:128, j, :], in_=tmpO[0:64, :]), ["tmpO"], ["OBT"])

                if stop == 'B':
                    return
                u, ut = load_unit(wba_d[:, 0:512].rearrange("(k p) n -> p k n", p=128), 4)
                u2, ut2 = load_unit(wba_d[:, 512:1024].rearrange("(k p) n -> p k n", p=128), 4)
                for m in range(8):
                    uu, uut = (u, ut) if m < 4 else (u2, ut2)
                    psm = PS[m % 2]
                    for k in range(4):
                        MM(psm[:], uu[:, k, (m % 4) * 128:(m % 4 + 1) * 128], OAT[:, k, :], k == 0, k == 3, [uut, "OAT"], [("ps", m % 2)])
                    CP("act", mrg[:, m, :], psm[:], [("ps", m % 2)], [("mrg", m), "qAT" if m < 4 else "qBT"])
                for hf in range(2):
                    u, ut = load_unit(wunit(win_d, 1536 + hf * 512))
                    for mm_ in range(4):
                        m = hf * 4 + mm_
                        psm = PS[m % 2]
                        for k in range(8):
                            MM(psm[:], u[:, k, mm_ * 128:(mm_ + 1) * 128], h0Tg[:, k, :], k == 0, k == 7, [ut, "h0Tg"], [("ps", m % 2)])
                        ACTF(t_sig, psm[:], AF.Sigmoid, [("ps", m % 2)], ["t_sig"])
                        TT("dve", mrg[:, m, :], mrg[:, m, :].bitcast(F32), t_sig, ALU.mult, [("mrg", m), "t_sig"], [("mrg", m)])
                for hf in range(2):
                    u, ut = load_unit(wbb_d[:, hf * 512:(hf + 1) * 512].rearrange("(k p) n -> p k n", p=128), 4)
                    u2, ut2 = load_unit(wunit(win_d, 2560 + hf * 512))
                    for mm_ in range(4):
                        m = hf * 4 + mm_
                        for k in range(4):
                            MM(PS[0][:], u[:, k, mm_ * 128:(mm_ + 1) * 128], OBT[:, k, :], k == 0, k == 3, [ut, "OBT"], [("ps", 0)])
                        for k in range(8):
                            MM(PS[1][:], u2[:, k, mm_ * 128:(mm_ + 1) * 128], h0Tg[:, k, :], k == 0, k == 7, [ut2, "h0Tg"], [("ps", 1)])
                        ACTF(t_sig, PS[1][:], AF.Sigmoid, [("ps", 1)], ["t_sig"])
                        TT("dve", t_a, PS[0][:], t_sig, ALU.mult, [("ps", 0), "t_sig"], ["t_a"])
                        TT("dve", mrg[:, m, :], mrg[:, m, :].bitcast(F32), t_a, ALU.add, [("mrg", m), "t_a"], [("mrg", m)])
                if stop == 'M':
                    return
                uo = []
                for hf in range(2):
                    uo.append(load_unit(wunit(wout_d, hf * 512)))
                mtoks = [("mrg", m) for m in range(8)]
                for j in range(4):
                    c = g * 4 + j
                    h0t = wk[:, j, :]
                    DMA("sp", h0t, H0_d[c * 128:(c + 1) * 128, :], [("H0", c)], [("wk", j)])
                    for hf in range(2):
                        u, ut = uo[hf]
                        psw = PS[hf]
                        for k in range(8):
                            MM(psw[:], mrg[:, k, j * 128:(j + 1) * 128], u[:, k, :], k == 0, k == 7, [ut] + mtoks, [("ps", hf)])
                        STT("dve", h0t[:, hf * 512:(hf + 1) * 512], h0t[:, hf * 512:(hf + 1) * 512], ALPHA, psw[:],
                            ALU.mult, ALU.add, [("wk", j), ("ps", hf)], [("wk", j)])
                    layer_norm(h0t, h0t, h0t, [("wk", j)], [("wk", j)], "ln1")
                    DMA("act", H1_d[c * 128:(c + 1) * 128, :], h0t, [("wk", j)], [("H1", c)])
                    if stage == 1:
                        DMA("act", dbg_d[c * 128:(c + 1) * 128, :], h0t, [("wk", j)], [("dbg", c)])


        if stop not in ("p1", "c0", "ln", "tr", "k", "kr", "v"):
            phase2()
        outs = []
        if stage >= 2:
            P.barrier()
            DMA("sp", wrt[:], wr_d.rearrange("(k p) n -> p k n", p=128), [], ["wrt"])
            DMA("sp", bdn[:], bd_d, [], ["bdn"])
            bgs = wk[0:32, 2, :]
            bus = wk[0:32, 3, :]
            DMA("sp", bgs, bg_d, [], ["bgs"])
            DMA("sp", bus, bu_d, [], ["bus"])
            for (src, stok, dstT, dtok) in ((bgs, "bgs", bgT, "bgT"), (bus, "bus", buT, "buT")):
                pst = PS[0]
                for m in range(8):
                    TR(pst[:, m * 32:(m + 1) * 32], src[:, m * 128:(m + 1) * 128], ident[0:32, 0:32], [stok], [("ps", 0)])
                CP("dve", dstT[:], pst[:, 0:256].rearrange("p (m e) -> p m e", m=8), [("ps", 0)], [dtok])
            P.op("pool", lambda e: e.memset(msum[:], 0.0), [], ["msum"])
            brt = smallb[:, 136:168]
            for c in range(NCH):
                h1t = wk[:, c % 2, :]
                htok = ("wk", c % 2)
                DMA("sp", h1t, H1_d[c * 128:(c + 1) * 128, :], [], [htok])
                for half in range(2):
                    pst = PS[2 + half]
                    for kk in range(4):
                        k = half * 4 + kk
                        TR(pst[:, kk * 128:(kk + 1) * 128], h1t[:, k * 128:(k + 1) * 128], ident[:], [htok], [("ps", 2 + half)])
                    CP("act" if half == 0 else "dve", (rcp if half == 0 else bcs)[:], pst[:], [("ps", 2 + half)],
                       ["h1T%d" % half])
                psl = PS[4]
                for k in range(8):
                    srcT = (rcp if k < 4 else bcs)[:, (k % 4) * 128:(k % 4 + 1) * 128]
                    MM(psl[:, 0:32], srcT, wrt[:, k, :], k == 0, k == 7, ["h1T0", "h1T1", "wrt"], [("ps", 4)])
                lg = rt[:, 0:32]
                TT("dve", lg, psl[:, 0:32], brt, ALU.add, [("ps", 4), "smallb"], ["lg"])
                mx = rt[:, 32:40]
                P.op("dve", lambda e, mx=mx, lg=lg: e.max(mx, lg), ["lg"], ["mx"])
                P.op("dve", lambda e, mx=mx, lg=lg: e.max_index(rti[:], mx, lg), ["lg", "mx"], ["rti"])
                negm = rt[:, 40:41]
                TS("dve", negm, mx[:, 0:1], -1.0, None, ALU.mult, None, ["mx"], ["negm"])
                e4 = rt[:, 44:48]
                esum = rt[:, 48:49]
                ACTF(e4, mx[:, 0:4], AF.Exp, ["mx", "negm"], ["e4", "esum"], bias=negm, accum=esum)
                P.op("dve", lambda e, esum=esum: e.reciprocal(rt[:, 49:50], esum), ["esum"], ["ersum"])
                TS("dve", wts[:, c, :], e4, rt[:, 49:50], None, ALU.mult, None, ["e4", "ersum"], ["wts"])
                mk = rt[:, 64:96]
                TS("dve", mk, lg, mx[:, 3:4], None, ALU.is_ge, None, ["lg", "mx"], ["mk"])
                psr = PS[5]
                if c > 0:
                    MM(psr[:, 0:32], onesM[:], msum[:], True, False, ["onesM", "msum"], [("ps", 5)])
                MM(psr[:, 0:32], tri[:], mk, c == 0, True, ["tri", "mk"], [("ps", 5)])
                TT("pool", msum[:], msum[:], mk, ALU.add, ["msum", "mk"], ["msum"])
                idxf = rt[:, 96:100]
                CP("dve", idxf, rti[:, 0:4], ["rti"], ["idxf"])
                oh = rt[:, 128:256].rearrange("p (k e) -> p k e", k=4)
                TT("dve", oh, iota[:].unsqueeze(1).to_broadcast([128, 4, 32]),
                   idxf.unsqueeze(2).to_broadcast([128, 4, 32]), ALU.is_equal, ["iota", "idxf"], ["oh"])
                pr = rt[:, 256:384].rearrange("p (k e) -> p k e", k=4)
                TT("dve", pr, oh, psr[:, 0:32].unsqueeze(1).to_broadcast([128, 4, 32]), ALU.mult, ["oh", ("ps", 5)], ["pr"])
                rk = rt[:, 100:104]
                P.op("dve", lambda e, rk=rk, pr=pr: e.reduce_sum(rk, pr, AX.X), ["pr"], ["rk"])
                posf = rt[:, 104:108]
                STT("dve", posf, idxf, float(CAP), rk, ALU.mult, ALU.add, ["idxf", "rk"], ["posf"])
                CP("dve", posi[:, c, :], posf, ["posf"], ["posi"])
                pw = rt[:, 384:512].rearrange("p (k e) -> p k e", k=4)
                TT("dve", pw, oh, wts[:, c, :].unsqueeze(2).to_broadcast([128, 4, 32]), ALU.mult, ["oh", "wts"], ["pw"])
                wdn = rt[:, 512:544]
                P.op("dve", lambda e, wdn=wdn, pw=pw: e.reduce_sum(wdn, pw.rearrange("p k e -> p e k"), AX.X), ["pw"], ["wdn"])
                TR(PS[6][0:32, 0:128], wdn, ident[:], ["wdn"], [("ps", 6)])
                CP("act", WdT[:, c * 128:(c + 1) * 128], PS[6][0:32, 0:128], [("ps", 6)], ["WdT"])
                for k4 in range(4):
                    P.dma("pool", lambda e, c=c, k4=k4, h1t=h1t: e.indirect_dma_start(
                        out=XS_d, out_offset=bass.IndirectOffsetOnAxis(ap=posi[:, c, k4:k4 + 1], axis=0),
                        in_=h1t, in_offset=None), [htok, "posi"], [("XS", c, k4)])

            P.barrier()
            ring5 = [UA[:, i * 4096:(i + 1) * 4096].rearrange("p (k n) -> p k n", k=8) for i in range(5)]
            rs5 = {"n": 0}

            def load5(src_ap):
                i = rs5["n"] % 5
                rs5["n"] += 1
                DMA("sp", ring5[i], src_ap, [], [("r5", i)])
                return ring5[i], ("r5", i)

            def eunit(w_ap, e, hf):
                return w_ap[e, :, hf * 512:(hf + 1) * 512].rearrange("(k p) n -> p k n", p=128)

            gTb = [gT, rcp[:, 0:CAP]]
            sgTb = [sgT, bcs[:, 0:CAP]]
            uTb = [uT, rt[:, 0:CAP]]
            t1Tb = [t1T, UC[:, 6656:6656 + CAP]]
            for e_ in range(NE):
                if e_ == 0:
                    DMA("act", Xe, XS_d[0:CAP, :].rearrange("(j p) d -> p j d", p=128), [], ["Xe"])
                ug = [None, None]
                uu_ = [None, None]
                ug[0] = load5(eunit(wg_d, e_, 0))
                uu_[0] = load5(eunit(wu_d, e_, 0))
                for k in range(8):
                    pst = PS[6 + (k % 2)]
                    for j in range(3):
                        TR(pst[:, j * 128:(j + 1) * 128], Xe[:, j, k * 128:(k + 1) * 128], ident[:], ["Xe"], [("ps", 6 + (k % 2))])
                    CP("act" if k % 2 == 0 else "dve", xTe[:, k, :], pst[:, 0:384], [("ps", 6 + (k % 2))], ["xTe"])
                if e_ + 1 < NE:
                    DMA("act", Xe, XS_d[(e_ + 1) * CAP:(e_ + 2) * CAP, :].rearrange("(j p) d -> p j d", p=128), [], ["Xe"])
                ug[1] = load5(eunit(wg_d, e_, 1))
                uu_[1] = load5(eunit(wu_d, e_, 1))
                for m in range(8):
                    hf, mm_ = m // 4, m % 4
                    (gu, gt), (uu, utk) = ug[hf], uu_[hf]
                    psg = PS[(m % 2) * 2]
                    psu = PS[(m % 2) * 2 + 1]
                    for k in range(8):
                        MM(psg[:, 0:CAP], gu[:, k, mm_ * 128:(mm_ + 1) * 128], xTe[:, k, :], k == 0, k == 7, [gt, "xTe"], [("ps", (m % 2) * 2)])
                    for k in range(8):
                        MM(psu[:, 0:CAP], uu[:, k, mm_ * 128:(mm_ + 1) * 128], xTe[:, k, :], k == 0, k == 7, [utk, "xTe"], [("ps", (m % 2) * 2 + 1)])
                    pb_ = m % 2
                    gT_, sgT_, uT_, t1T_ = gTb[pb_], sgTb[pb_], uTb[pb_], t1Tb[pb_]
                    TS("dve", gT_, psg[:, 0:CAP], bgT[:, m, e_:e_ + 1], 7.0, ALU.add, ALU.min, [("ps", (m % 2) * 2), "bgT"], [("gT", pb_)])
                    ACTF(sgT_, gT_, AF.Sigmoid, [("gT", pb_)], [("sgT", pb_)], scale=1.702)
                    TS("dve", uT_, psu[:, 0:CAP], buT[:, m, e_:e_ + 1], 7.0, ALU.add, ALU.min, [("ps", (m % 2) * 2 + 1), "buT"], [("uT", pb_)])
                    TS("dve", uT_, uT_, -7.0, 1.0, ALU.max, ALU.add, [("uT", pb_)], [("uT", pb_)])
                    TT("dve", t1T_, gT_, sgT_, ALU.mult, [("gT", pb_), ("sgT", pb_)], [("t1T", pb_)])
                    TT("dve", actT[:, m, :], t1T_, uT_, ALU.mult, [("t1T", pb_), ("uT", pb_)], ["actT"])
                ud = [load5(eunit(wd_d, e_, 0)), load5(eunit(wd_d, e_, 1))]
                for t in range(3):
                    yt = ytile[t % 2]
                    ytok = ("yt", t % 2)
                    for hf in range(2):
                        u, ut = ud[hf]
                        psy = PS[4 + hf]
                        for k in range(8):
                            MM(psy[:], actT[:, k, t * 128:(t + 1) * 128], u[:, k, :], k == 0, k == 7, [ut, "actT"], [("ps", 4 + hf)])
                        CP("act", yt[:, hf * 512:(hf + 1) * 512], psy[:], [("ps", 4 + hf)], [ytok])
                    DMA("act", YS_d[e_ * CAP + t * 128:e_ * CAP + (t + 1) * 128, :], yt, [ytok], [("YS", e_, t)])

            P.barrier()
            DMA("sp", lnG[:], lnp_d[4:5, :].to_broadcast([128, D]), [], ["lnG"])
            DMA("sp", lnB[:], lnp_d[5:6, :].to_broadcast([128, D]), [], ["lnB"])
            gsets = [
                [UC[:, 0:1024], UC[:, 1024:2048], UC[:, 2048:3072], UC[:, 3072:4096]],
                [UC[:, 4096:5120], UC[:, 5120:6144], UC[:, 6144:7168], wk[:, 2, :]],
            ]
            for c in range(NCH):
                par = c % 2
                h1t = wk[:, par, :]
                htok = ("wk", par)
                DMA("sp", h1t, H1_d[c * 128:(c + 1) * 128, :], [], [htok])
                gts = []
                for k4 in range(4):
                    gt_ = gsets[par][k4]
                    gk_ = ("ga", par, k4)
                    P.dma("pool", lambda e, c=c, k4=k4, gt_=gt_: e.indirect_dma_start(
                        out=gt_, out_offset=None, in_=YS_d,
                        in_offset=bass.IndirectOffsetOnAxis(ap=posi[:, c, k4:k4 + 1], axis=0)), ["posi"], [gk_])
                    gts.append((gt_, gk_))
                for hf in range(2):
                    MM(PS[2 * par + hf][:], WdT[:, c * 128:(c + 1) * 128], bdn[:, hf * 512:(hf + 1) * 512], True, True,
                       ["WdT", "bdn"], [("ps", 2 * par + hf)])
                acc, atok = gts[0]
                TS("dve", acc, acc, wts[:, c, 0:1], None, ALU.mult, None, [atok, "wts"], [atok])
                for k4 in range(1, 4):
                    STT("dve", acc, gts[k4][0], wts[:, c, k4:k4 + 1], acc, ALU.mult, ALU.add,
                        [gts[k4][1], "wts", atok], [atok])
                for hf in range(2):
                    TT("dve", acc[:, hf * 512:(hf + 1) * 512], acc[:, hf * 512:(hf + 1) * 512], PS[2 * par + hf][:], ALU.add,
                       [atok, ("ps", 2 * par + hf)], [atok])
                STT("dve", acc, h1t, ALPHA, acc, ALU.mult, ALU.add, [htok, atok], [atok])
                layer_norm(acc, acc, acc, [atok], [atok], "ln2")
                outs.append(DMA("act", out_d[c * 128:(c + 1) * 128, :], acc, [atok], [("out", c)]))
        if stage < 3:
            fw = {"act": list(P.dma_ops["act"][-10:]), "sp": list(P.dma_ops["sp"][-10:]), "pool": list(P.dma_ops["pool"][-10:])}
        else:
            fw = {"act": outs}
        P.emit(fw)
    return nc


def _prep_inputs(inp):
    f = lambda a: np.ascontiguousarray(np.asarray(a, dtype=np.float32))
    w_in = f(inp["w_in"])[0]
    qa, ka, va, qg, kg, vg, ga, gb = np.split(w_in, np.cumsum([512, 128, 128, 512, 128, 128, 1024])[:], axis=1)

    def qperm(q):
        cols = []
        for j in range(4):
            cols.append(q[:, j * 64:(j + 1) * 64])
            cols.append(q[:, (j + 4) * 64:(j + 5) * 64])
        return np.concatenate(cols, axis=1)

    w_inp = np.ascontiguousarray(np.concatenate([qperm(qa), qperm(qg), ka, kg, va, vg, ga, gb], axis=1))

    def bperm(w):
        rows = []
        for j in range(4):
            rows.append(w[j * 64:(j + 1) * 64])
            rows.append(w[(j + 4) * 64:(j + 5) * 64])
        return np.ascontiguousarray(np.concatenate(rows, axis=0))

    lnp = np.stack([f(inp["ln0_g"]), f(inp["ln0_b"]), f(inp["ln1_g"])[0], f(inp["ln1_b"])[0],
                    f(inp["ln2_g"])[0], f(inp["ln2_b"])[0]], axis=0)
    small = np.concatenate([f(inp["a_sink"])[0], f(inp["b_q_norm"])[0], f(inp["b_k_norm"])[0],
                            f(inp["b_router"])[0]])[None, :]
    shared = {
        "lnp": np.ascontiguousarray(lnp), "w_in": w_inp, "small": np.ascontiguousarray(small),
        "w_ba": bperm(f(inp["w_branch_a"])[0]), "w_bb": bperm(f(inp["w_branch_b"])[0]),
        "w_out": f(inp["w_out"])[0], "w_router": f(inp["w_router"])[0],
        "w_gate": f(inp["w_gate"])[0], "w_up": f(inp["w_up"])[0], "w_down": f(inp["w_down"])[0],
        "b_gate": f(inp["b_gate"])[0], "b_up": f(inp["b_up"])[0], "b_down": f(inp["b_down"])[0],
    }
    shared.update(_consts())
    return shared


def kernel(**inputs):
    shared = _prep_inputs(inputs)
    x = np.asarray(inputs["x"], dtype=np.float32)
    n = x.shape[0]
    nc = build_nc(3)
    in_maps = []
    for b in range(n):
        m = dict(shared)
        m["x"] = np.ascontiguousarray(x[b])
        in_maps.append(m)
    res = run_bass_kernel_spmd(nc, in_maps, core_ids=list(range(n)))
    return np.stack([np.asarray(r["out"], dtype=np.float32) for r in res.results], axis=0)
```
